# Optimizing a Trainium2 kernel written in Bass

```python
import math
import jax, jax.numpy as jnp
from jax import lax
import numpy as np

D_MODEL = 1024
BATCH = 4
SEQ = 4096
DEPTH = 4

HEAD_DIM = 64
Q_BLOCK = 128
RMS_EPS = 1e-6
NEG_INF = -1e30
A_HEADS = 8
A_LATENT = 256
IDX_HEADS = 8
IDX_DIM = 64
TOPK_MAX = 256
B_HEADS = 4
C_HEADS = 16
REL_BUCKETS = 32
REL_MAX_DIST = 128
REL_HEADS = A_HEADS + B_HEADS
PEER_HEADS = 8
PEER_QDIM = 256
PEER_NKEYS = 128
PEER_EXPERTS = PEER_NKEYS * PEER_NKEYS
PEER_TOPK = 16
PEER_CHUNK = 128
EVEN_SIZES = (A_HEADS * HEAD_DIM, A_LATENT, IDX_HEADS * IDX_DIM, IDX_DIM, IDX_HEADS,
              B_HEADS * 2 * HEAD_DIM, B_HEADS * 2 * HEAD_DIM, B_HEADS * 2 * HEAD_DIM)
EVEN_IN = A_HEADS * HEAD_DIM + A_LATENT + IDX_HEADS * IDX_DIM + IDX_DIM + IDX_HEADS + 3 * B_HEADS * 2 * HEAD_DIM
EVEN_MIX = A_HEADS * HEAD_DIM + B_HEADS * 2 * HEAD_DIM
ODD_SIZES = (C_HEADS * HEAD_DIM, C_HEADS * HEAD_DIM, C_HEADS * HEAD_DIM, C_HEADS)
ODD_IN = 3 * C_HEADS * HEAD_DIM + C_HEADS
ODD_MIX = C_HEADS * HEAD_DIM

kernel_name = "hybrid_dsa_diff_fox_peer_trunk"


def rmsnorm(x, g):
    xf = x.astype(jnp.float32)
    y = xf * lax.rsqrt(jnp.mean(xf * xf, axis=-1, keepdims=True) + RMS_EPS)
    return (y * g.astype(jnp.float32)).astype(x.dtype)


def split_cols(p, sizes):
    points = np.cumsum(np.array(sizes))[:-1].tolist()
    return jnp.split(p, points, axis=-1)


def rel_bucket(dist):
    max_exact = REL_BUCKETS // 2
    d = jnp.maximum(dist, 0)
    df = jnp.maximum(d, 1).astype(jnp.float32)
    large = max_exact + (jnp.log(df / max_exact) / math.log(REL_MAX_DIST / max_exact)
                         * (REL_BUCKETS - max_exact)).astype(jnp.int32)
    large = jnp.minimum(large, REL_BUCKETS - 1)
    return jnp.where(d < max_exact, d, large)


def sweep_blocks(fn, batch, seq_len):
    n = seq_len // Q_BLOCK
    out = lax.map(fn, jnp.arange(n, dtype=jnp.int32) * Q_BLOCK)
    out = jnp.moveaxis(out, 0, 1)
    return out.reshape((batch, seq_len) + out.shape[3:])


def dsa_attention(q, ckv, q_idx, k_idx, w_idx, w_uk, w_uv, bias_a):
    B, S = q.shape[0], q.shape[1]
    topk = min(TOPK_MAX, S // 4)
    key_pos = jnp.arange(S, dtype=jnp.int32)
    scale = HEAD_DIM ** -0.5
    idx_scale = IDX_DIM ** -0.5
    q_lat = jnp.einsum('bthd,chd->bthc', q, w_uk)

    def block(start):
        qpos = start + jnp.arange(Q_BLOCK, dtype=jnp.int32)
        qi = lax.dynamic_slice_in_dim(q_idx, start, Q_BLOCK, axis=1)
        wi = lax.dynamic_slice_in_dim(w_idx, start, Q_BLOCK, axis=1)
        ql = lax.dynamic_slice_in_dim(q_lat, start, Q_BLOCK, axis=1)
        li = jnp.einsum('bthd,bsd->bths', qi, k_idx).astype(jnp.float32) * idx_scale
        iscore = jnp.einsum('bths,bth->bts', jax.nn.relu(li), wi.astype(jnp.float32))
        causal = key_pos[None, :] <= qpos[:, None]
        iscore = jnp.where(causal[None], iscore, -jnp.inf)
        _, sel = lax.top_k(iscore, topk)
        kv_sel = jax.vmap(lambda lat, ix: lat[ix])(ckv, sel)
        dist = qpos[None, :, None] - sel
        s = jnp.einsum('bthc,btkc->bthk', ql, kv_sel).astype(jnp.float32) * scale
        s = s + jnp.moveaxis(bias_a[rel_bucket(dist)], -1, 2).astype(jnp.float32)
        s = jnp.where((dist >= 0)[:, :, None, :], s, NEG_INF)
        p = jax.nn.softmax(s, axis=-1).astype(ckv.dtype)
        o_lat = jnp.einsum('bthk,btkc->bthc', p, kv_sel)
        o = jnp.einsum('bthc,chd->bthd', o_lat, w_uv)
        return o.reshape(B, Q_BLOCK, A_HEADS * HEAD_DIM)

    return sweep_blocks(block, B, S)


def diff_attention(q1, q2, k1, k2, v, lam, lam_init, subln_g, bias_b):
    B, S = q1.shape[0], q1.shape[1]
    key_pos = jnp.arange(S, dtype=jnp.int32)
    scale = HEAD_DIM ** -0.5

    def block(start):
        qpos = start + jnp.arange(Q_BLOCK, dtype=jnp.int32)
        causal = key_pos[None, :] <= qpos[:, None]
        bias = jnp.moveaxis(bias_b[rel_bucket(qpos[:, None] - key_pos[None, :])], -1, 0)
        bias = bias.astype(jnp.float32)

        def probs(q, k):
            qb = lax.dynamic_slice_in_dim(q, start, Q_BLOCK, axis=1)
            s = jnp.einsum('bthd,bshd->bhts', qb, k).astype(jnp.float32) * scale + bias
            return jax.nn.softmax(jnp.where(causal, s, NEG_INF), axis=-1)

        a = probs(q1, k1) - lam * probs(q2, k2)
        return jnp.einsum('bhts,bshe->bthe', a.astype(v.dtype), v)

    o = sweep_blocks(block, B, S)
    o = rmsnorm(o, subln_g) * (1.0 - lam_init)
    return o.reshape(B, S, B_HEADS * 2 * HEAD_DIM)


def forgetting_attention(q, k, v, f_logit):
    B, S = q.shape[0], q.shape[1]
    key_pos = jnp.arange(S, dtype=jnp.int32)
    scale = HEAD_DIM ** -0.5
    cum = jnp.moveaxis(jnp.cumsum(jax.nn.log_sigmoid(f_logit.astype(jnp.float32)), axis=1), -1, 1)

    def block(start):
        qpos = start + jnp.arange(Q_BLOCK, dtype=jnp.int32)
        causal = key_pos[None, :] <= qpos[:, None]
        qb = lax.dynamic_slice_in_dim(q, start, Q_BLOCK, axis=1)
        cq = lax.dynamic_slice_in_dim(cum, start, Q_BLOCK, axis=2)
        s = jnp.einsum('bthd,bshd->bhts', qb, k).astype(jnp.float32) * scale
        s = s + cq[..., :, None] - cum[..., None, :]
        p = jax.nn.softmax(jnp.where(causal, s, NEG_INF), axis=-1).astype(v.dtype)
        return jnp.einsum('bhts,bshd->bthd', p, v)

    return sweep_blocks(block, B, S).reshape(B, S, C_HEADS * HEAD_DIM)


def even_mixer(h, w_in, w_out, kv_norm, w_uk, w_uv, lam_vec, subln_g, rel_bias, lam_init):
    B, S, _ = h.shape
    q_a, ckv, q_i, k_i, w_i, q_b, k_b, v_b = split_cols(h @ w_in, EVEN_SIZES)
    q_a = q_a.reshape(B, S, A_HEADS, HEAD_DIM)
    ckv = rmsnorm(ckv, kv_norm)
    q_i = q_i.reshape(B, S, IDX_HEADS, IDX_DIM)
    w_i = w_i * (IDX_HEADS ** -0.5)
    o_a = dsa_attention(q_a, ckv, q_i, k_i, w_i, w_uk, w_uv, rel_bias[:, :A_HEADS])
    q_b = q_b.reshape(B, S, B_HEADS, 2, HEAD_DIM)
    k_b = k_b.reshape(B, S, B_HEADS, 2, HEAD_DIM)
    v_b = v_b.reshape(B, S, B_HEADS, 2 * HEAD_DIM)
    lv = lam_vec.astype(jnp.float32)
    lam = jnp.exp(jnp.sum(lv[0] * lv[1])) - jnp.exp(jnp.sum(lv[2] * lv[3])) + lam_init
    o_b = diff_attention(q_b[..., 0, :], q_b[..., 1, :], k_b[..., 0, :], k_b[..., 1, :],
                         v_b, lam, lam_init, subln_g, rel_bias[:, A_HEADS:])
    return jnp.concatenate([o_a, o_b], axis=-1) @ w_out


def odd_mixer(h, w_in, b_forget, w_out):
    B, S, _ = h.shape
    q, k, v, f = split_cols(h @ w_in, ODD_SIZES)
    shp = (B, S, C_HEADS, HEAD_DIM)
    o = forgetting_attention(q.reshape(shp), k.reshape(shp), v.reshape(shp), f + b_forget)
    return o @ w_out


def peer_ffn(h, w_q, sub_keys, u, v):
    B, S, D = h.shape
    T = B * S
    ht = h.reshape(T, D)
    q = (ht @ w_q).reshape(T, PEER_HEADS, 2, PEER_QDIM // 2)
    s = jnp.einsum('thpd,pnd->thpn', q, sub_keys).astype(jnp.float32)
    top_s, top_i = lax.top_k(s, PEER_TOPK)
    cand_s = top_s[:, :, 0, :, None] + top_s[:, :, 1, None, :]
    cand_i = top_i[:, :, 0, :, None] * PEER_NKEYS + top_i[:, :, 1, None, :]
    best_s, best_j = lax.top_k(cand_s.reshape(T, PEER_HEADS, -1), PEER_TOPK)
    experts = jnp.take_along_axis(cand_i.reshape(T, PEER_HEADS, -1), best_j, axis=-1)
    gates = jax.nn.softmax(best_s, axis=-1)
    n_chunks = T // PEER_CHUNK
    hk = PEER_HEADS * PEER_TOPK

    def chunk(args):
        xc, ec, gc = args
        act = jax.nn.gelu(jnp.einsum('cd,ced->ce', xc, u[ec]))
        return jnp.einsum('ce,ced->cd', gc.astype(xc.dtype) * act, v[ec])

    out = lax.map(chunk, (ht.reshape(n_chunks, PEER_CHUNK, D),
                          experts.reshape(n_chunks, PEER_CHUNK, hk),
                          gates.reshape(n_chunks, PEER_CHUNK, hk)))
    return out.reshape(B, S, D)


def setup_inputs(seed: int = 0) -> dict:
    key = jax.random.key(seed)
    ks = jax.random.split(key, 24)
    n_even = (DEPTH + 1) // 2
    n_odd = DEPTH // 2

    def nrm(k, shape, scale):
        return jax.random.normal(k, shape, jnp.float32) * scale

    return {
        "x": nrm(ks[0], (BATCH, SEQ, D_MODEL), 1.0),
        "c": nrm(ks[1], (BATCH, D_MODEL), 1.0),
        "rel_bias": nrm(ks[2], (REL_BUCKETS, REL_HEADS), 0.5),
        "ada_w": nrm(ks[3], (DEPTH, D_MODEL, 6 * D_MODEL), 0.5 * D_MODEL ** -0.5),
        "ada_b": nrm(ks[4], (DEPTH, 6 * D_MODEL), 0.02),
        "norm_mix": 1.0 + nrm(ks[5], (DEPTH, D_MODEL), 0.02),
        "norm_ffn": 1.0 + nrm(ks[6], (DEPTH, D_MODEL), 0.02),
        "norm_final": 1.0 + nrm(ks[7], (D_MODEL,), 0.02),
        "even_w_in": nrm(ks[8], (n_even, D_MODEL, EVEN_IN), D_MODEL ** -0.5),
        "even_w_out": nrm(ks[9], (n_even, EVEN_MIX, D_MODEL), EVEN_MIX ** -0.5),
        "a_kv_norm": 1.0 + nrm(ks[10], (n_even, A_LATENT), 0.02),
        "a_w_uk": nrm(ks[11], (n_even, A_LATENT, A_HEADS, HEAD_DIM), A_LATENT ** -0.5),
        "a_w_uv": nrm(ks[12], (n_even, A_LATENT, A_HEADS, HEAD_DIM), A_LATENT ** -0.5),
        "b_lambda": nrm(ks[13], (n_even, 4, HEAD_DIM), 0.1),
        "b_subln": 1.0 + nrm(ks[14], (n_even, 2 * HEAD_DIM), 0.02),
        "odd_w_in": nrm(ks[15], (n_odd, D_MODEL, ODD_IN), D_MODEL ** -0.5),
        "odd_b_forget": 2.0 + nrm(ks[16], (n_odd, C_HEADS), 0.5),
        "odd_w_out": nrm(ks[17], (n_odd, ODD_MIX, D_MODEL), ODD_MIX ** -0.5),
        "peer_w_q": nrm(ks[18], (DEPTH, D_MODEL, PEER_HEADS * PEER_QDIM), D_MODEL ** -0.5),
        "peer_sub_keys": nrm(ks[19], (DEPTH, 2, PEER_NKEYS, PEER_QDIM // 2), (PEER_QDIM // 2) ** -0.5),
        "peer_u": nrm(ks[20], (DEPTH, PEER_EXPERTS, D_MODEL), D_MODEL ** -0.5),
        "peer_v": nrm(ks[21], (DEPTH, PEER_EXPERTS, D_MODEL), PEER_HEADS ** -0.5),
    }


def reference(x, c, rel_bias, ada_w, ada_b, norm_mix, norm_ffn, norm_final,
              even_w_in, even_w_out, a_kv_norm, a_w_uk, a_w_uv, b_lambda, b_subln,
              odd_w_in, odd_b_forget, odd_w_out,
              peer_w_q, peer_sub_keys, peer_u, peer_v):
    c_act = jax.nn.silu(c)
    for layer in range(DEPTH):
        mod = (c_act @ ada_w[layer] + ada_b[layer])[:, None, :]
        sh1, sc1, g1, sh2, sc2, g2 = jnp.split(mod, 6, axis=-1)
        h = rmsnorm(x, norm_mix[layer]) * (1.0 + sc1) + sh1
        if layer % 2 == 0:
            e = layer // 2
            lam_init = 0.8 - 0.6 * math.exp(-0.3 * layer)
            y = even_mixer(h, even_w_in[e], even_w_out[e], a_kv_norm[e], a_w_uk[e], a_w_uv[e],
                           b_lambda[e], b_subln[e], rel_bias, lam_init)
        else:
            o = layer // 2
            y = odd_mixer(h, odd_w_in[o], odd_b_forget[o], odd_w_out[o])
        x = x + g1 * y
        h = rmsnorm(x, norm_ffn[layer]) * (1.0 + sc2) + sh2
        x = x + g2 * peer_ffn(h, peer_w_q[layer], peer_sub_keys[layer], peer_u[layer], peer_v[layer])
    return rmsnorm(x, norm_final)
```

```python
import math
import numpy as np
import concourse.bass as bass
import concourse.mybir as mybir
from concourse.bass_utils import run_bass_kernel_spmd
from contextlib import ExitStack

F32 = mybir.dt.float32
BF16 = mybir.dt.bfloat16
I32 = mybir.dt.int32
U32 = mybir.dt.uint32
AF = mybir.ActivationFunctionType
ALU = mybir.AluOpType
AX = mybir.AxisListType

EPOCH = 30000
NDMASEM = 12

S = 4096
D = 1024
NT = S // 128
DEPTH = 4
EPS = 1e-6


class Buf:
    __slots__ = ("t", "lw", "rd", "name")

    def __init__(self, t, name=""):
        self.t = t
        self.lw = None
        self.rd = {}
        self.name = name

    def __getitem__(self, idx):
        return self.t[idx]


class KB:
    def __init__(self, nc):
        self.nc = nc
        self.es = ExitStack()
        self.eng = {"pe": nc.tensor, "act": nc.scalar, "dve": nc.vector,
                    "pool": nc.gpsimd, "sp": nc.sync}
        self.sems = {}
        self.cur = {}
        self.seen = {e: {} for e in self.eng}
        self.nsem = 0
        self.prog = {e: [] for e in self.eng}
        for e in self.eng:
            self._new_epoch(e)
        self.dma = {}
        for e in ("sp", "pool"):
            pool = []
            for i in range(NDMASEM):
                k = f"d_{e}_{i}"
                self.sems[k] = nc.alloc_semaphore(name=k)
                pool.append([k, 0])
            self.dma[e] = [pool, 0]
        self.ninstr = {e: 0 for e in self.eng}
        self.cache = {}
        self.pcache = {}
        self.arena = None
        self.bump = 0

    def emit(self):
        nc = self.nc
        with nc.Block() as block:
            for e, dec in (("pe", block.tensor), ("act", block.scalar), ("dve", block.vector),
                           ("pool", block.gpsimd), ("sp", block.sync)):
                prog = self.prog[e]
                eng = self.eng[e]

                def body(_x, prog=prog, eng=eng):
                    for it in prog:
                        if it[0] == "w":
                            eng.wait_ge(it[1], it[2])
                        else:
                            ins = it[1]()
                            ins.then_inc(it[2], it[3])
                dec(body)

    def _new_epoch(self, e):
        k = f"s_{e}_{self.nsem}"
        self.nsem += 1
        self.sems[k] = self.nc.alloc_semaphore(name=k)
        self.cur[e] = [k, 0]

    ARENA = 50000

    def sb(self, name, shape, dt, persist=False):
        if name in self.cache:
            return self.cache[name]
        if name in self.pcache:
            return self.pcache[name]
        if persist:
            t = self.es.enter_context(self.nc.sbuf_tensor(name, list(shape), dt))
            b = Buf(t, name)
            self.pcache[name] = b
            return b
        if self.arena is None:
            self.arena = self.es.enter_context(self.nc.sbuf_tensor("arena", [128, self.ARENA], F32))
        isz = 4 if dt in (F32, U32, I32) else 2
        nel = int(np.prod(shape[1:]))
        nw = (nel * isz + 31) // 32 * 8
        off = self.bump
        self.bump += nw
        assert self.bump <= self.ARENA, f"arena overflow at {name}: {self.bump * 4}"
        ap = self.arena[:, off:off + nw]
        if dt != F32:
            ap = ap.bitcast(dt)
        ap = ap[0:shape[0], 0:nel]
        if len(shape) == 3:
            ap = ap.rearrange("p (a b) -> p a b", a=shape[1])
        elif len(shape) == 4:
            ap = ap.rearrange("p (a b c) -> p a b c", a=shape[1], b=shape[2])
        b = Buf(ap, name)
        self.cache[name] = b
        return b

    def phase_reset(self):
        self.cache = {}
        self.bump = 0

    def ps(self, name, shape, dt=F32):
        if name in self.cache:
            return self.cache[name]
        t = self.es.enter_context(self.nc.psum_tensor(name, list(shape), dt))
        b = Buf(t, name)
        self.cache[name] = b
        return b

    def dram(self, name, shape, dt):
        t = self.nc.dram_tensor(name, list(shape), dt, kind="Internal")
        return Buf(t.ap(), name)

    def _wait(self, e, ev):
        if ev is None:
            return
        k, v = ev
        if e == "pe" and k.startswith("s_pe_"):
            return
        if self.seen[e].get(k, 0) >= v:
            return
        self.prog[e].append(("w", self.sems[k], v))
        self.seen[e][k] = v

    def _deps(self, e, reads, writes):
        for b in reads:
            self._wait(e, b.lw)
        for b in writes:
            self._wait(e, b.lw)
            for k, v in list(b.rd.items()):
                self._wait(e, (k, v))

    def _mark(self, ev, reads, writes):
        k, v = ev
        for b in reads:
            if b.rd.get(k, 0) < v:
                b.rd[k] = v
        for b in writes:
            b.lw = ev
            b.rd = {}

    def op(self, e, fn, reads=(), writes=()):
        self._deps(e, reads, writes)
        self.ninstr[e] += 1
        c = self.cur[e]
        self.prog[e].append(("i", fn, self.sems[c[0]], 1))
        c[1] += 1
        self._mark((c[0], c[1]), reads, writes)
        if c[1] >= EPOCH:
            self._new_epoch(e)

    def dma_op(self, e, fn, reads=(), writes=()):
        pool, idx = self.dma[e]
        slot = pool[idx % NDMASEM]
        self.dma[e][1] += 1
        if slot[1] > 0:
            self._wait(e, (slot[0], slot[1]))
        self._deps(e, reads, writes)
        self.ninstr[e] += 1
        self.prog[e].append(("i", fn, self.sems[slot[0]], 16))
        slot[1] += 16
        self._mark((slot[0], slot[1]), reads, writes)

    def finish(self, bufs):
        for b in bufs:
            self._wait("sp", b.lw)


class Prog:
    def __init__(self, layers, final_norm, dbg_peer_only=False):
        self.layers = layers
        self.final_norm = final_norm
        self.dbg_peer_only = dbg_peer_only
        nc = bass.Bass("TRN2", target_bir_lowering=False)
        self.nc = nc
        self.kb = KB(nc)
        kb = self.kb
        di = {}

        def inp(name, shape, dt=F32):
            di[name] = Buf(nc.dram_tensor("i_" + name, list(shape), dt, kind="ExternalInput").ap(), name)
            return di[name]
        self.di = di
        inp("x", [S, D])
        inp("ccol", [128, 8])
        inp("ident", [128, 128])
        inp("ada_w", [DEPTH, D, 6 * D])
        inp("ada_b", [DEPTH, 1, 6 * D])
        inp("norm_mix", [DEPTH, 1, D])
        inp("norm_ffn", [DEPTH, 1, D])
        inp("norm_final", [1, D])
        inp("peer_w_q", [DEPTH, D, 2048])
        inp("peer_skT", [DEPTH, 2, 128, 128])
        inp("peer_uv", [DEPTH * 16384, 2 * D])
        inp("iota16", [128, 16])
        inp("odd_w_in", [2, D, 3088])
        inp("odd_b_forget", [2, 16, 1])
        inp("odd_w_out", [2, D, D])
        inp("sel127", [128, 128])
        inp("even_w_in", [2, D, 2888])
        inp("even_w_out", [2, D, D])
        inp("a_kv_norm", [2, 1, 256])
        inp("ukT", [2, 8, 64, 256])
        inp("a_w_uv", [2, 256, 512])
        inp("b_lambda", [2, 4, 64])
        inp("b_subln", [2, 128, 1])
        inp("b31rep", [128, 12])
        inp("biasT", [128, 24, 128])
        inp("negtri", [128, 128])
        inp("tri", [128, 128])
        self.out = Buf(nc.dram_tensor("out", [S, D], F32, kind="ExternalOutput").ap(), "out")
        self.MODBC = kb.dram("modbc", [DEPTH, 128, 6 * D], F32)
        self.XA = kb.dram("xa", [S, D], F32)
        self.XB = kb.dram("xb", [S, D], F32)
        self.UV16 = kb.dram("uv16", [DEPTH * 16384, 2 * D], BF16)
        self.UV16L = [Buf(self.UV16.t, f"uv16_{l}") for l in range(DEPTH)]
        self._uvdone = set()
        self.QC = kb.dram("qc", [8, 128, S], BF16)
        self.KC = kb.dram("kc", [8, 128, S], BF16)
        self.VC = kb.dram("vc", [S, D], BF16)
        self.OT = kb.dram("ot", [16, 64, S], BF16)
        self.CQ = kb.dram("cq", [16, S], BF16)
        self.QLAT = kb.dram("qlat", [16, 128, S], BF16)
        self.CKV = kb.dram("ckv", [S, 256], BF16)
        self.CKVT = kb.dram("ckvt", [2, 128, S], BF16)
        self.QI = kb.dram("qi", [8, 64, S], BF16)
        self.KI = kb.dram("ki", [64, S], BF16)
        self.WI = kb.dram("wi", [S, 8], F32)
        self.QB = kb.dram("qb", [4, 128, S], BF16)
        self.KBd = kb.dram("kbd", [4, 128, S], BF16)
        self.VB = kb.dram("vb", [S, 512], BF16)
        self.MT = kb.dram("mt", [32, 128, S], BF16)
        self.OTA = kb.dram("ota", [8, 64, S], BF16)
        self.OTB = kb.dram("otb", [4, 128, S], BF16)
        self.PS = [kb.ps(f"ps{i}", [128, 512], F32) for i in range(7)]
        self.PSB = kb.ps("psb", [128, 1024], BF16)
        self.ident = kb.sb("ident", [128, 128], F32, persist=True)
        self.identb = kb.sb("identb", [128, 128], BF16, persist=True)
        self.ones_row = kb.sb("ones_row", [1, 128], F32, persist=True)
        self.eps_col = kb.sb("eps_col", [128, 1], F32, persist=True)
        kb.dma_op("sp", lambda: nc.sync.dma_start(out=self.ident[:, :], in_=di["ident"][:, :]),
                  reads=[di["ident"]], writes=[self.ident])
        kb.op("dve", lambda: nc.vector.tensor_copy(out=self.identb[:, :], in_=self.ident[:, :]),
              reads=[self.ident], writes=[self.identb])
        kb.op("dve", lambda: nc.vector.memset(self.ones_row[:, :], 1.0), writes=[self.ones_row])
        kb.op("dve", lambda: nc.vector.memset(self.eps_col[:, :], EPS), writes=[self.eps_col])

    def barrier(self, reset=True):
        kb = self.kb
        evs = []
        for e in kb.eng:
            k, c = kb.cur[e]
            if c > 0:
                evs.append((k, c))
        for e in ("sp", "pool"):
            for slot in kb.dma[e][0]:
                if slot[1] > 0:
                    evs.append((slot[0], slot[1]))
        for e in kb.eng:
            for ev in evs:
                if ev[0].startswith(f"s_{e}_"):
                    continue
                kb._wait(e, ev)
        if reset:
            kb.phase_reset()

    def convert_uv(self, l):
        if l in self._uvdone:
            return
        self._uvdone.add(l)
        nc, kb = self.nc, self.kb
        k = f"cv_{l}"
        kb.sems[k] = nc.alloc_semaphore(name=k)
        src = self.di["peer_uv"]
        dst = self.UV16.t
        nd = 32
        rows = 16384 // nd
        for i in range(nd):
            r0 = l * 16384 + i * rows
            kb.prog["pool"].append(("i", (lambda r0=r0: nc.gpsimd.dma_start(out=dst[r0:r0 + rows, :], in_=src[r0:r0 + rows, :])),
                                    kb.sems[k], 16))
        self.UV16L[l].lw = (k, 16 * nd)

    def mm(self, out_b, out_ap, l_b, l_ap, r_b, r_ap, start, stop):
        nc = self.nc
        self.kb.op("pe", lambda: nc.tensor.matmul(out_ap, lhsT=l_ap, rhs=r_ap, start=start, stop=stop),
                   reads=[l_b, r_b], writes=[out_b])

    def load(self, dst_b, dst_ap, src_b, src_ap, q="sp"):
        nc = self.nc
        eng = nc.sync if q == "sp" else nc.gpsimd
        self.kb.dma_op(q, lambda: eng.dma_start(out=dst_ap, in_=src_ap), reads=[src_b], writes=[dst_b])

    def phase_mods(self):
        nc, kb, di = self.nc, self.kb, self.di
        ccol = kb.sb("ccol", [128, 8], F32)
        cact = kb.sb("cact", [128, 8], F32)
        cbc = Buf(kb.sb("xo", [128, D], F32)[:, :].rearrange("p (k t) -> p k t", k=8), "cbc")
        self.load(ccol, ccol[:, :], di["ccol"], di["ccol"][:, :])
        kb.op("act", lambda: nc.scalar.activation(out=cact[:, :], in_=ccol[:, :], func=AF.Silu),
              reads=[ccol], writes=[cact])
        kb.op("dve", lambda: nc.vector.tensor_copy(out=cbc[:, :, :], in_=cact[:, :].unsqueeze(2).to_broadcast([128, 8, 128])),
              reads=[cact], writes=[cbc])
        brow = kb.sb("brow", [1, 512], F32)
        gall = kb.sb("gbuf_all", [128, 6176], F32)
        wst = [Buf(gall[:, i * 3088:(i + 1) * 3088], f"wst{i}") for i in range(2)]
        mo = Buf(kb.sb("qT", [128, 16, 256], F32)[:, :, :].rearrange("p a b -> p (a b)"), "mo")
        n = 0
        for l in self.layers:
            for half in range(2):
                c0 = half * 3072
                for kc in range(8):
                    w = wst[n % 2]
                    n += 1
                    self.load(w, w[:, :3072], di["ada_w"], di["ada_w"][l, kc * 128:(kc + 1) * 128, c0:c0 + 3072])
                    for j in range(6):
                        self.mm(self.PS[j], self.PS[j][:, :], cbc, cbc[:, kc, :], w, w[:, j * 512:(j + 1) * 512],
                                start=(kc == 0), stop=False)
                for j in range(6):
                    self.load(brow, brow[:, :], di["ada_b"], di["ada_b"][l, :, c0 + j * 512:c0 + (j + 1) * 512])
                    self.mm(self.PS[j], self.PS[j][:, :], self.ones_row, self.ones_row[:, :],
                            brow, brow[:, :], start=False, stop=True)
                for j in range(6):
                    pj = self.PS[j]
                    if j % 2 == 0:
                        kb.op("act", lambda pj=pj, j=j: nc.scalar.copy(out=mo[:, j * 512:(j + 1) * 512], in_=pj[:, :]),
                              reads=[pj], writes=[mo])
                    else:
                        kb.op("dve", lambda pj=pj, j=j: nc.vector.tensor_copy(out=mo[:, j * 512:(j + 1) * 512], in_=pj[:, :]),
                              reads=[pj], writes=[mo])
                self.load(self.MODBC, self.MODBC[l, :, c0:c0 + 3072], mo, mo[:, 0:3072])

    def load_modvecs(self, l, which, norm_name):
        nc, kb, di = self.nc, self.kb, self.di
        gv = kb.sb("gvec", [128, D], F32)
        sv = kb.sb("shvec", [128, D], F32)
        gate = kb.sb("gatevec", [128, D], F32)
        nv = kb.sb("xo", [128, D], F32)
        base = which * 3 * D
        self.load(sv, sv[:, :], self.MODBC, self.MODBC[l, :, base:base + D])
        self.load(gv, gv[:, :], self.MODBC, self.MODBC[l, :, base + D:base + 2 * D])
        self.load(gate, gate[:, :], self.MODBC, self.MODBC[l, :, base + 2 * D:base + 3 * D])
        self.load(nv, nv[:, :], di[norm_name], di[norm_name][l, :, :].partition_broadcast(128))
        kb.op("dve", lambda: nc.vector.scalar_tensor_tensor(out=gv[:, :], in0=gv[:, :], scalar=1.0, in1=nv[:, :],
                                                            op0=ALU.add, op1=ALU.mult),
              reads=[gv, nv], writes=[gv])
        return gv, sv, gate

    def norm_tile(self, xt, gv, sv, htok, h16):
        nc, kb = self.nc, self.kb
        junk = kb.sb("junk", [128, D], BF16)
        ssq = kb.sb("ssq", [128, 1], F32)
        rstd = kb.sb("rstd", [128, 1], F32)
        kb.op("act", lambda: nc.scalar.activation(out=junk[:, :], in_=xt[:, :], func=AF.Square, accum_out=ssq[:, :]),
              reads=[xt], writes=[junk, ssq])
        kb.op("act", lambda: nc.scalar.activation(out=rstd[:, :], in_=ssq[:, :], func=AF.Sqrt, scale=1.0 / D,
                                                  bias=self.eps_col[:, :]),
              reads=[ssq, self.eps_col], writes=[rstd])
        kb.op("dve", lambda: nc.vector.reciprocal(out=rstd[:, :], in_=rstd[:, :]), reads=[rstd], writes=[rstd])
        kb.op("dve", lambda: nc.vector.scalar_tensor_tensor(out=htok[:, :], in0=xt[:, :], scalar=rstd[:, :], in1=gv[:, :],
                                                            op0=ALU.mult, op1=ALU.mult),
              reads=[xt, rstd, gv], writes=[htok])
        kb.op("dve", lambda: nc.vector.tensor_tensor(out=htok[:, :], in0=htok[:, :], in1=sv[:, :], op=ALU.add),
              reads=[htok, sv], writes=[htok])
        if h16 is not None:
            kb.op("act", lambda: nc.scalar.copy(out=h16[:, :], in_=htok[:, :]), reads=[htok], writes=[h16])

    def transpose_tile(self, h16, hT, col0):
        nc, kb = self.nc, self.kb
        for kc in range(8):
            kb.op("pe", lambda kc=kc: nc.tensor.transpose(self.PSB[:, kc * 128:(kc + 1) * 128],
                                                          h16[:, kc * 128:(kc + 1) * 128], self.identb[:, :]),
                  reads=[h16, self.identb], writes=[self.PSB])
        kb.op("dve", lambda: nc.vector.tensor_copy(out=hT[:, :, col0:col0 + 128],
                                                   in_=self.PSB[:, :].rearrange("p (k t) -> p k t", k=8)),
              reads=[self.PSB], writes=[hT])

    def load_w16(self, w16, src_b, src_ap_fn, ncols, stage=None):
        nc, kb = self.nc, self.kb
        if stage is None:
            gall = kb.sb("gbuf_all", [128, 6176], F32)
            st = [Buf(gall[:, i * 3088:(i + 1) * 3088], f"wst{i}") for i in range(2)]
        else:
            qT_ = kb.sb("qT", [128, 16, 256], F32)
            st = [Buf(qT_[:, i * 8:(i + 1) * 8, :].rearrange("p a b -> p (a b)"), f"wstg{i}") for i in range(2)]
        for kc in range(8):
            s = st[kc % 2]
            self.load(s, s[:, :ncols], src_b, src_ap_fn(kc))
            if kc % 2 == 0:
                kb.op("dve", lambda s=s, kc=kc: nc.vector.tensor_copy(out=w16[:, kc, :ncols], in_=s[:, :ncols]),
                      reads=[s], writes=[w16])
            else:
                kb.op("act", lambda s=s, kc=kc: nc.scalar.copy(out=w16[:, kc, :ncols], in_=s[:, :ncols]),
                      reads=[s], writes=[w16])


    def inproj_blocks(self, XIN, gv, sv, w16, fm_specs, tm_specs, hT, extra=None):
        nc, kb = self.nc, self.kb
        xts = [kb.sb(f"xt{i}", [128, D], F32) for i in range(2)]
        hts = [kb.sb(f"htok{i}", [128, D], F32) for i in range(2)]
        h16 = [kb.sb(f"h16_{i}", [128, D], BF16) for i in range(2)]
        n = 0
        for blk in range(S // 256):
            for tt in range(2):
                ti = blk * 2 + tt
                xt = xts[tt]
                self.load(xt, xt[:, :], XIN, XIN[ti * 128:(ti + 1) * 128, :])
                self.norm_tile(xt, gv, sv, hts[tt], h16[tt])
                self.transpose_tile(h16[tt], hT, tt * 128)
            for (col0, M, fn) in fm_specs:
                pb = self.PS[n % 3]
                n += 1
                for kc in range(8):
                    self.mm(pb, pb[0:M, 0:256], w16, w16[:, kc, col0:col0 + M], hT, hT[:, kc, :],
                            start=(kc == 0), stop=(kc == 7))
                fn(pb, blk)
            for tt in range(2):
                ti = blk * 2 + tt
                for (col0, ncols, fn) in tm_specs:
                    pb = self.PS[3 + n % 3]
                    n += 1
                    for kc in range(8):
                        self.mm(pb, pb[:, 0:ncols], hT, hT[:, kc, tt * 128:(tt + 1) * 128], w16, w16[:, kc, col0:col0 + ncols],
                                start=(kc == 0), stop=(kc == 7))
                    fn(pb, ti)
            if extra is not None:
                extra(blk)

    def outproj(self, l, XIN, XOUT, gate, w_out_b, w_out_ap, chunks, OT_loader):
        nc, kb = self.nc, self.kb
        nch = len(chunks)
        wo = kb.sb("w16", [128, 8, 3088], BF16)
        wov = Buf(wo[:, :, :].rearrange("p a b -> p (a b)"), "wov")
        gall = kb.sb("gbuf_all", [128, 6176], F32)
        st = [Buf(gall[:, i * 1024:(i + 1) * 1024], f"wos{i}") for i in range(4)]
        for ci, (K, r0) in enumerate(chunks):
            sbuf = st[ci % 4]
            self.load(sbuf, sbuf[0:K, :], w_out_b, w_out_ap(r0, K))
            if ci % 2 == 0:
                kb.op("dve", lambda sbuf=sbuf, ci=ci, K=K: nc.vector.tensor_copy(out=wov[0:K, ci * 1024:(ci + 1) * 1024], in_=sbuf[0:K, :]),
                      reads=[sbuf], writes=[wov])
            else:
                kb.op("act", lambda sbuf=sbuf, ci=ci, K=K: nc.scalar.copy(out=wov[0:K, ci * 1024:(ci + 1) * 1024], in_=sbuf[0:K, :]),
                      reads=[sbuf], writes=[wov])
        self.barrier(reset=False)
        xts = [kb.sb(f"xt{i}", [128, D], F32) for i in range(2)]
        xo = [kb.sb(f"htok{i}", [128, D], F32) for i in range(2)]
        for ti in range(NT):
            xt = xts[ti % 2]
            self.load(xt, xt[:, :], XIN, XIN[ti * 128:(ti + 1) * 128, :])
            obaps = OT_loader(ti)
            for half in range(2):
                pb = self.PS[(ti % 2) * 2 + half]
                for ci, (K, r0) in enumerate(chunks):
                    self.mm(pb, pb[:, :], obaps[ci][0], obaps[ci][1], wov, wov[0:K, ci * 1024 + half * 512: ci * 1024 + (half + 1) * 512],
                            start=(ci == 0), stop=(ci == nch - 1))
            o = xo[ti % 2]
            for half in range(2):
                pb = self.PS[(ti % 2) * 2 + half]
                kb.op("dve", lambda pb=pb, o=o, half=half: nc.vector.tensor_tensor(
                    out=o[:, half * 512:(half + 1) * 512], in0=pb[:, :], in1=gate[:, half * 512:(half + 1) * 512], op=ALU.mult),
                    reads=[pb, gate], writes=[o])
            kb.op("dve", lambda o=o, xt=xt: nc.vector.tensor_tensor(out=o[:, :], in0=o[:, :], in1=xt[:, :], op=ALU.add),
                  reads=[o, xt], writes=[o])
            self.load(XOUT, XOUT[ti * 128:(ti + 1) * 128, :], o, o[:, :])

    def attn_block(self, I, KT, kt_aps, Q, q_aps, V, v_aps, dvs, bias_ap, mask_fn, ones_b, extra=None, clamp_diag=False, unit_mask_fn=None):
        nc, kb = self.nc, self.kb
        pts = [kb.sb(f"pt{i}", [128, 512], BF16) for i in range(3)]
        rz = kb.sb("rz", [128, 512], F32)
        nv = len(dvs)
        osb = [[kb.sb(f"osb{c}_{i}", [128, 512], BF16) for c in range(nv)] for i in range(2)]
        n = self._attn_n
        par = self._attn_par
        self._attn_par ^= 1
        pOs = [self.PS[2 + c] for c in range(nv)]
        pZ = self.PS[4]
        jmax = 4 * I + 3

        def emit_sc(j):
            nonlocal n
            i0 = max(4 * I, j)
            c0 = (i0 - 4 * I) * 128
            psc = self.PS[n % 2]
            pt = pts[n % 3]
            n += 1
            ks = kt_aps(j)
            qs = q_aps(I * 512 + c0, (I + 1) * 512)
            for kk in range(len(ks)):
                self.mm(psc, psc[:, c0:512], KT, ks[kk], Q, qs[kk], start=(kk == 0), stop=(kk == len(ks) - 1 and extra is None))
            if extra is not None:
                lb, lap, rb, rapf = extra
                self.mm(psc, psc[:, c0:512], lb, lap, rb, rapf(I * 512 + c0, (I + 1) * 512), start=False, stop=True)
            return (j, i0, c0, psc, pt)

        nxt = emit_sc(0)
        for j in range(jmax + 1):
            (_, i0, c0, psc, pt) = nxt
            b = bias_ap(I, j)
            breads = [psc] + ([b[0]] if isinstance(b, tuple) else [])
            bval = b[1] if isinstance(b, tuple) else b
            a0 = c0
            if clamp_diag and j >= 4 * I:
                tmpc = kb.sb("tmpc", [128, 128], F32)
                kb.op("dve", lambda psc=psc, c0=c0, bval=bval: nc.vector.tensor_scalar(
                    out=tmpc[:, :], in0=psc[:, c0:c0 + 128], scalar1=bval, scalar2=40.0, op0=ALU.add, op1=ALU.min),
                    reads=breads, writes=[tmpc])
                kb.op("act", lambda pt=pt, c0=c0: nc.scalar.activation(out=pt[:, c0:c0 + 128], in_=tmpc[:, :], func=AF.Exp),
                      reads=[tmpc], writes=[pt])
                a0 = c0 + 128
            if a0 < 512:
                kb.op("act", lambda psc=psc, pt=pt, a0=a0, bval=bval: nc.scalar.activation(
                    out=pt[:, a0:512], in_=psc[:, a0:512], func=AF.Exp, bias=bval),
                    reads=breads, writes=[pt])
            if unit_mask_fn is not None:
                unit_mask_fn(pt, c0, I, j)
            for i in range(i0, 4 * I + 4):
                mask_fn(pt, (i - 4 * I) * 128, i, j)
            if j < jmax:
                nxt = emit_sc(j + 1)
            vs_ = v_aps(j)
            for c in range(nv):
                self.mm(pOs[c], pOs[c][0:dvs[c], c0:512], V, vs_[c], pt, pt[:, c0:512], start=(j == 0), stop=(j == jmax))
            self.mm(pZ, pZ[:, c0:512], ones_b, ones_b[:, :], pt, pt[:, c0:512], start=(j == 0), stop=(j == jmax))
        kb.op("dve", lambda: nc.vector.reciprocal(out=rz[:, :], in_=pZ[:, :]), reads=[pZ], writes=[rz])
        outs = []
        for c in range(nv):
            ob = osb[par][c]
            kb.op("dve", lambda ob=ob, c=c: nc.vector.tensor_tensor(out=ob[0:dvs[c], :], in0=pOs[c][0:dvs[c], :], in1=rz[0:dvs[c], :], op=ALU.mult),
                  reads=[pOs[c], rz], writes=[ob])
            outs.append(ob)
        self._attn_n = n
        return outs


    def prep_ctiles(self):
        if getattr(self, "_ctiles", None) is not None:
            return
        nc, kb, di = self.nc, self.kb, self.di
        ct = kb.sb("ctiles", [128, 24, 128], BF16, persist=True)
        b31 = kb.sb("b31", [128, 12], F32, persist=True)
        nb31 = kb.sb("nb31", [128, 12], F32, persist=True)
        self.load(b31, b31[:, :], di["b31rep"], di["b31rep"][:, :])
        kb.op("dve", lambda: nc.vector.tensor_scalar(out=nb31[:, :], in0=b31[:, :], scalar1=-1.0, scalar2=None, op0=ALU.mult),
              reads=[b31], writes=[nb31])
        tri32 = kb.sb("tri32", [128, 128], F32)
        self.load(tri32, tri32[:, :], di["tri"], di["tri"][:, :])
        bt = kb.sb("btst", [128, 24, 128], F32)
        self.load(bt, bt[:, :, :], di["biasT"], di["biasT"][:, :, :])
        for h in range(12):
            for dl in range(2):
                k = h * 2 + dl
                kb.op("act", lambda k=k, h=h: nc.scalar.activation(out=bt[:, k, :], in_=bt[:, k, :], func=AF.Exp, bias=nb31[:, h:h + 1]),
                      reads=[bt, nb31], writes=[bt])
                if dl == 0:
                    kb.op("dve", lambda k=k: nc.vector.tensor_tensor(out=ct[:, k, :], in0=bt[:, k, :], in1=tri32[:, :], op=ALU.mult),
                          reads=[bt, tri32], writes=[ct])
                else:
                    kb.op("dve", lambda k=k: nc.vector.tensor_copy(out=ct[:, k, :], in_=bt[:, k, :]), reads=[bt], writes=[ct])
        self._ctiles = (ct, b31)
        self.barrier()

    def phase_even(self, l, XIN, XOUT):
        nc, kb, di = self.nc, self.kb, self.di
        e = l // 2
        lam_init = 0.8 - 0.6 * math.exp(-0.3 * l)
        self.prep_ctiles()
        ct, b31 = self._ctiles
        gv, sv, gate = self.load_modvecs(l, 0, "norm_mix")
        w16 = kb.sb("w16", [128, 8, 3088], BF16)
        self.load_w16(w16, di["even_w_in"], lambda kc: di["even_w_in"][e, kc * 128:(kc + 1) * 128, :], 2888)
        gall = kb.sb("gbuf_all", [128, 6176], F32)
        ukst = Buf(gall[0:64, 0:2048].rearrange("p (h c) -> p h c", h=8), "ukst")
        ukT = kb.sb("ukT", [64, 8, 256], BF16)
        self.barrier(reset=False)
        self.load(ukst, ukst[:, :, :], di["ukT"], di["ukT"][e, :, :, :].rearrange("h d c -> d h c"))
        kb.op("dve", lambda: nc.vector.tensor_copy(out=ukT[:, :, :], in_=ukst[:, :, :]), reads=[ukst], writes=[ukT])
        kvn = kb.sb("kvn", [128, 256], F32)
        self.load(kvn, kvn[:, :], di["a_kv_norm"], di["a_kv_norm"][e, :, :].partition_broadcast(128))
        self.barrier(reset=False)
        hT = kb.sb("hT", [128, 8, 256], BF16)
        QLAT, CKV, CKVT, QI, KI, WI, QB, KBd, VB = (self.QLAT, self.CKV, self.CKVT, self.QI, self.KI, self.WI,
                                                     self.QB, self.KBd, self.VB)
        qah = [kb.sb(f"qah{i}", [64, 256], BF16) for i in range(2)]
        qlst = [kb.sb(f"qlst{i}", [128, 16, 256], BF16) for i in range(2)]
        qist = [kb.sb(f"qist{i}", [64, 8, 256], BF16) for i in range(2)]
        kist = [kb.sb(f"kist{i}", [64, 256], BF16) for i in range(2)]
        qbst = [kb.sb(f"qbst{i}", [128, 4, 256], BF16) for i in range(2)]
        kbst = [kb.sb(f"kbst{i}", [128, 4, 256], BF16) for i in range(2)]
        vbst = [kb.sb(f"vbst{i}", [128, 512], BF16) for i in range(2)]
        ckst = [kb.sb(f"ckst{i}", [128, 256], BF16) for i in range(2)]
        cktst = [kb.sb(f"cktst{i}", [128, 2, 128], BF16) for i in range(2)]
        wist = [kb.sb(f"wist{i}", [128, 8], F32) for i in range(2)]
        fm, tm = [], []
        for h in range(8):
            def fqa(pb, blk, h=h):
                qa = qah[h % 2]
                ql = qlst[blk % 2]
                kb.op("act", lambda: nc.scalar.copy(out=qa[:, :], in_=pb[0:64, 0:256]), reads=[pb], writes=[qa])
                for cc in range(2):
                    p2 = self.PS[6]
                    self.mm(p2, p2[:, 0:256], ukT, ukT[:, h, cc * 128:(cc + 1) * 128], qa, qa[:, :], start=True, stop=True)
                    kb.op("dve", lambda cc=cc, p2=p2: nc.vector.tensor_scalar(out=ql[:, h * 2 + cc, :], in0=p2[:, 0:256], scalar1=0.125,
                                                                              scalar2=None, op0=ALU.mult), reads=[p2], writes=[ql])
                if h == 7:
                    self.load(QLAT, QLAT[:, :, blk * 256:(blk + 1) * 256].rearrange("m p t -> p m t"), ql, ql[:, :, :])
            fm.append((h * 64, 64, fqa))
        for h in range(8):
            def fqi(pb, blk, h=h):
                q = qist[blk % 2]
                kb.op("act", lambda: nc.scalar.copy(out=q[:, h, :], in_=pb[0:64, 0:256]), reads=[pb], writes=[q])
                if h == 7:
                    self.load(QI, QI[:, :, blk * 256:(blk + 1) * 256].rearrange("h p t -> p h t"), q, q[:, :, :])
            fm.append((768 + h * 64, 64, fqi))

        def fki(pb, blk):
            k = kist[blk % 2]
            kb.op("dve", lambda: nc.vector.tensor_copy(out=k[:, :], in_=pb[0:64, 0:256]), reads=[pb], writes=[k])
            self.load(KI, KI[:, blk * 256:(blk + 1) * 256], k, k[:, :])
        fm.append((1280, 64, fki))
        for h in range(4):
            def fqb(pb, blk, h=h):
                q = qbst[blk % 2]
                kb.op("act", lambda: nc.scalar.activation(out=q[:, h, :], in_=pb[:, 0:256], func=AF.Copy, scale=0.125),
                      reads=[pb], writes=[q])
                if h == 3:
                    self.load(QB, QB[:, :, blk * 256:(blk + 1) * 256].rearrange("h p t -> p h t"), q, q[:, :, :])
            fm.append((1352 + h * 128, 128, fqb))
        for h in range(4):
            def fkb(pb, blk, h=h):
                k = kbst[blk % 2]
                kb.op("dve", lambda: nc.vector.tensor_copy(out=k[:, h, :], in_=pb[:, 0:256]), reads=[pb], writes=[k])
                if h == 3:
                    self.load(KBd, KBd[:, :, blk * 256:(blk + 1) * 256].rearrange("h p t -> p h t"), k, k[:, :, :])
            fm.append((1864 + h * 128, 128, fkb))
        junkc = kb.sb("junkc", [128, 256], BF16)
        ssqc = kb.sb("ssqc", [128, 1], F32)
        ckf = kb.sb("ckf", [128, 256], F32)

        def fckv(pb, ti):
            ck = ckst[ti % 2]
            ckt = cktst[ti % 2]
            kb.op("act", lambda: nc.scalar.activation(out=junkc[:, :], in_=pb[:, 0:256], func=AF.Square, accum_out=ssqc[:, :]),
                  reads=[pb], writes=[junkc, ssqc])
            kb.op("act", lambda: nc.scalar.activation(out=ssqc[:, :], in_=ssqc[:, :], func=AF.Sqrt, scale=1.0 / 256, bias=self.eps_col[:, :]),
                  reads=[ssqc, self.eps_col], writes=[ssqc])
            kb.op("dve", lambda: nc.vector.reciprocal(out=ssqc[:, :], in_=ssqc[:, :]), reads=[ssqc], writes=[ssqc])
            kb.op("dve", lambda: nc.vector.scalar_tensor_tensor(out=ck[:, :], in0=pb[:, 0:256], scalar=ssqc[:, :], in1=kvn[:, :],
                                                                op0=ALU.mult, op1=ALU.mult), reads=[pb, ssqc, kvn], writes=[ck])
            self.load(CKV, CKV[ti * 128:(ti + 1) * 128, :], ck, ck[:, :])
            for cc in range(2):
                kb.op("pe", lambda cc=cc: nc.tensor.transpose(self.PSB[:, cc * 128:(cc + 1) * 128], ck[:, cc * 128:(cc + 1) * 128], self.identb[:, :]),
                      reads=[ck, self.identb], writes=[self.PSB])
            kb.op("act", lambda: nc.scalar.copy(out=ckt[:, :, :], in_=self.PSB[:, 0:256].rearrange("p (c t) -> p c t", c=2)),
                  reads=[self.PSB], writes=[ckt])
            self.load(CKVT, CKVT[:, :, ti * 128:(ti + 1) * 128].rearrange("c p t -> p c t"), ckt, ckt[:, :, :])
        tm.append((512, 256, fckv))

        def fwi(pb, ti):
            w = wist[ti % 2]
            kb.op("dve", lambda: nc.vector.tensor_scalar(out=w[:, :], in0=pb[:, 0:8], scalar1=(8.0 ** -0.5) * 0.125, scalar2=None,
                                                         op0=ALU.mult), reads=[pb], writes=[w])
            self.load(WI, WI[ti * 128:(ti + 1) * 128, :], w, w[:, :])
        tm.append((1344, 8, fwi))

        def fvb(pb, ti):
            v = vbst[ti % 2]
            kb.op("act", lambda: nc.scalar.copy(out=v[:, :], in_=pb[:, :]), reads=[pb], writes=[v])
            self.load(VB, VB[ti * 128:(ti + 1) * 128, :], v, v[:, :])
        tm.append((2376, 512, fvb))
        self.inproj_blocks(XIN, gv, sv, w16, fm, tm, hT)
        self.barrier()
        MT = self.MT
        kiT = kb.sb("kiT", [64, S], BF16)
        self.load(kiT, kiT[:, :], KI, KI[:, :])
        negtri = kb.sb("negtri", [128, 128], F32)
        self.load(negtri, negtri[:, :], di["negtri"], di["negtri"][:, :])
        isc = [kb.sb(f"isc{i}", [128, S], F32) for i in range(2)]
        tmpw = [kb.sb(f"tmpw{i}", [128, 512], F32) for i in range(3)]
        qit = [kb.sb(f"qit{i}", [64, 8, 128], BF16) for i in range(2)]
        wit = [kb.sb(f"wit{i}", [128, 8], F32) for i in range(2)]
        mk = [kb.sb(f"mk{i}", [128, S], BF16) for i in range(2)]
        mts = [kb.sb(f"mts{i}", [128, 8, 128], BF16) for i in range(2)]
        jk = kb.sb("jk", [128, S], BF16)
        sc_ = {n_: kb.sb(f"bs_{n_}", [128, 1], F32) for n_ in ("lo", "hi", "mid", "cnt", "ge", "d1", "d2")}
        n = 0
        nm = 0
        for i in range(NT):
            L = (i + 1) * 128
            q = qit[i % 2]
            w = wit[i % 2]
            sc = isc[i % 2]
            self.load(q, q[:, :, :], QI, QI[:, :, i * 128:(i + 1) * 128].rearrange("h p t -> p h t"))
            self.load(w, w[:, :], WI, WI[i * 128:(i + 1) * 128, :])
            for kbk in range((L + 511) // 512):
                a = kbk * 512
                b_ = min(L, a + 512)
                wd = b_ - a
                for h in range(8):
                    pb = self.PS[n % 4]
                    n += 1
                    self.mm(pb, pb[:, 0:wd], q, q[:, h, :], kiT, kiT[:, a:b_], start=True, stop=True)
                    if h == 0:
                        kb.op("dve", lambda pb=pb, a=a, b_=b_, wd=wd, sc=sc, w=w: nc.vector.tensor_scalar(
                            out=sc[:, a:b_], in0=pb[:, 0:wd], scalar1=0.0, scalar2=w[:, 0:1], op0=ALU.max, op1=ALU.mult),
                            reads=[pb, w], writes=[sc])
                    else:
                        t_ = tmpw[n % 3]
                        kb.op("dve", lambda pb=pb, wd=wd, t_=t_, w=w, h=h: nc.vector.tensor_scalar(
                            out=t_[:, 0:wd], in0=pb[:, 0:wd], scalar1=0.0, scalar2=w[:, h:h + 1], op0=ALU.max, op1=ALU.mult),
                            reads=[pb, w], writes=[t_])
                        kb.op("pool", lambda a=a, b_=b_, wd=wd, t_=t_, sc=sc: nc.gpsimd.tensor_tensor(
                            out=sc[:, a:b_], in0=sc[:, a:b_], in1=t_[:, 0:wd], op=ALU.add), reads=[sc, t_], writes=[sc])
            lo, hi, mid, cnt, ge, d1, d2 = (sc_[k_] for k_ in ("lo", "hi", "mid", "cnt", "ge", "d1", "d2"))
            kb.op("dve", lambda sc=sc, L=L: nc.vector.tensor_reduce(out=lo[:, :], in_=sc[:, 0:L], axis=AX.X, op=ALU.min), reads=[sc], writes=[lo])
            kb.op("dve", lambda: nc.vector.tensor_scalar(out=lo[:, :], in0=lo[:, :], scalar1=-1.0, scalar2=None, op0=ALU.add), reads=[lo], writes=[lo])
            kb.op("dve", lambda sc=sc, L=L: nc.vector.tensor_tensor(out=sc[:, L - 128:L], in0=sc[:, L - 128:L], in1=negtri[:, :], op=ALU.add),
                  reads=[sc, negtri], writes=[sc])
            kb.op("dve", lambda sc=sc, L=L: nc.vector.tensor_reduce(out=hi[:, :], in_=sc[:, 0:L], axis=AX.X, op=ALU.max), reads=[sc], writes=[hi])
            kb.op("dve", lambda: nc.vector.tensor_scalar(out=hi[:, :], in0=hi[:, :], scalar1=1e-3, scalar2=None, op0=ALU.add), reads=[hi], writes=[hi])
            for it in range(18 if i >= 2 else 0):
                kb.op("dve", lambda: nc.vector.tensor_tensor(out=mid[:, :], in0=lo[:, :], in1=hi[:, :], op=ALU.add), reads=[lo, hi], writes=[mid])
                kb.op("dve", lambda: nc.vector.tensor_scalar(out=mid[:, :], in0=mid[:, :], scalar1=0.5, scalar2=None, op0=ALU.mult), reads=[mid], writes=[mid])
                kb.op("dve", lambda sc=sc, L=L: nc.vector.tensor_scalar(out=jk[:, 0:L], in0=sc[:, 0:L], scalar1=mid[:, 0:1], scalar2=0.0,
                                                                        op0=ALU.is_ge, op1=ALU.add, accum_out=cnt[:, :]),
                      reads=[sc, mid], writes=[jk, cnt])
                kb.op("dve", lambda: nc.vector.tensor_scalar(out=ge[:, :], in0=cnt[:, :], scalar1=255.5, scalar2=None, op0=ALU.is_ge), reads=[cnt], writes=[ge])
                kb.op("dve", lambda: nc.vector.tensor_tensor(out=d1[:, :], in0=mid[:, :], in1=lo[:, :], op=ALU.subtract), reads=[mid, lo], writes=[d1])
                kb.op("dve", lambda: nc.vector.tensor_tensor(out=d2[:, :], in0=hi[:, :], in1=mid[:, :], op=ALU.subtract), reads=[hi, mid], writes=[d2])
                kb.op("dve", lambda: nc.vector.scalar_tensor_tensor(out=lo[:, :], in0=d1[:, :], scalar=ge[:, 0:1], in1=lo[:, :], op0=ALU.mult, op1=ALU.add),
                      reads=[d1, ge, lo], writes=[lo])
                kb.op("dve", lambda: nc.vector.scalar_tensor_tensor(out=hi[:, :], in0=d2[:, :], scalar=ge[:, 0:1], in1=mid[:, :], op0=ALU.mult, op1=ALU.add),
                      reads=[d2, ge, mid], writes=[hi])
            m_ = mk[i % 2]
            kb.op("dve", lambda sc=sc, L=L, m_=m_: nc.vector.tensor_scalar(out=m_[:, 0:L], in0=sc[:, 0:L], scalar1=lo[:, 0:1], scalar2=None, op0=ALU.is_ge),
                  reads=[sc, lo], writes=[m_])
            for j0 in range(0, i + 1, 8):
                j1 = min(i + 1, j0 + 8)
                mt = mts[nm % 2]
                nm += 1
                for j in range(j0, j1):
                    kb.op("pe", lambda j=j, j0=j0, m_=m_: nc.tensor.transpose(self.PSB[:, (j - j0) * 128:(j - j0 + 1) * 128], m_[:, j * 128:(j + 1) * 128], self.identb[:, :]),
                          reads=[m_, self.identb], writes=[self.PSB])
                nj = j1 - j0
                kb.op("act", lambda mt=mt, nj=nj: nc.scalar.copy(out=mt[:, 0:nj, :], in_=self.PSB[:, 0:nj * 128].rearrange("p (j t) -> p j t", j=nj)),
                      reads=[self.PSB], writes=[mt])
                self.load(MT, MT[j0:j1, :, i * 128:(i + 1) * 128].rearrange("j p t -> p j t"), mt, mt[:, 0:nj, :])
        self.barrier()
        onesk = kb.sb("onesk", [128, 128], BF16)
        kb.op("dve", lambda: nc.vector.memset(onesk[:, :], 1.0), writes=[onesk])
        ckT = kb.sb("ckT", [128, 2, S], BF16)
        self.load(ckT, ckT[:, :, :], CKVT, CKVT[:, :, :].rearrange("c p t -> p c t"))
        ckv = kb.sb("ckvV", [128, 32, 256], BF16)
        for jj in range(4):
            self.load(ckv, ckv[:, jj * 8:(jj + 1) * 8, :], CKV, CKV[jj * 1024:(jj + 1) * 1024, :].rearrange("(j p) d -> p j d", p=128))
        uvst = kb.sb("uvst", [128, 2, 512], F32)
        uv = kb.sb("uv", [128, 2, 512], BF16)
        self.load(uvst, uvst[:, :, :], di["a_w_uv"], di["a_w_uv"][e, :, :].rearrange("(c p) n -> p c n", p=128))
        kb.op("dve", lambda: nc.vector.tensor_copy(out=uv[:, :, :], in_=uvst[:, :, :]), reads=[uvst], writes=[uv])
        Ql = [kb.sb(f"Ql{i}", [128, 2, S], BF16) for i in range(2)]
        mtl = [kb.sb(f"mtl{i}", [128, 512], BF16) for i in range(3)]
        oas = [kb.sb(f"oas{i}", [64, 512], BF16) for i in range(2)]
        OTA, OTB = self.OTA, self.OTB
        self._attn_n = 0
        self._attn_par = 0
        nmt = 0
        for h in range(8):
            q = Ql[h % 2]
            self.load(q, q[:, :, :], QLAT, QLAT[h * 2:h * 2 + 2, :, :].rearrange("c p t -> p c t"))
            for I in range(8):

                def unit_mask(pt, c0, I_, j):
                    nonlocal nmt
                    mt = mtl[nmt % 3]
                    nmt += 1
                    self.load(mt, mt[:, c0:512], MT, MT[j, :, I_ * 512 + c0:(I_ + 1) * 512])
                    kb.op("pool", lambda: nc.gpsimd.tensor_tensor(out=pt[:, c0:512], in0=pt[:, c0:512], in1=mt[:, c0:512], op=ALU.mult),
                          reads=[pt, mt], writes=[pt])

                def mask_fn(pt, cs, i, j, h=h):
                    if i - j in (0, 1):
                        k = h * 2 + (i - j)
                        kb.op("dve", lambda: nc.vector.tensor_tensor(out=pt[:, cs:cs + 128], in0=pt[:, cs:cs + 128], in1=ct[:, k, :], op=ALU.mult),
                              reads=[pt, ct], writes=[pt])
                outs = self.attn_block(I, ckT, lambda j: [ckT[:, 0, j * 128:(j + 1) * 128], ckT[:, 1, j * 128:(j + 1) * 128]],
                                       q, lambda a, b, q=q: [q[:, 0, a:b], q[:, 1, a:b]],
                                       ckv, lambda j: [ckv[:, j, 0:128], ckv[:, j, 128:256]], [128, 128],
                                       lambda i, j, h=h: (b31, b31[:, h:h + 1]), mask_fn, onesk, unit_mask_fn=unit_mask)
                p5 = self.PS[5]
                for cc in range(2):
                    self.mm(p5, p5[0:64, :], uv, uv[:, cc, h * 64:(h + 1) * 64], outs[cc], outs[cc][:, :], start=(cc == 0), stop=(cc == 1))
                oa = oas[I % 2]
                kb.op("act", lambda oa=oa, p5=p5: nc.scalar.copy(out=oa[:, :], in_=p5[0:64, :]), reads=[p5], writes=[oa])
                self.load(OTA, OTA[h, :, I * 512:(I + 1) * 512], oa, oa[:, :])
        self.barrier()
        onesk = kb.sb("onesk", [128, 128], BF16)
        kb.op("dve", lambda: nc.vector.memset(onesk[:, :], 1.0), writes=[onesk])
        ones32 = kb.sb("ones32", [128, 128], F32)
        kb.op("dve", lambda: nc.vector.memset(ones32[:, :], 1.0), writes=[ones32])
        lamt = kb.sb("lamt", [128, 4, 64], F32)
        self.load(lamt, lamt[:, :, :].rearrange("p a b -> p (a b)"), di["b_lambda"], di["b_lambda"][e:e + 1, :, :].rearrange("o a b -> o (a b)").partition_broadcast(128))
        lp = kb.sb("lp", [128, 2, 64], F32)
        ls = kb.sb("ls", [128, 2], F32)
        neglam = kb.sb("neglam", [128, 1], F32)
        kb.op("dve", lambda: nc.vector.tensor_tensor(out=lp[:, :, :], in0=lamt[:, 0::2, :], in1=lamt[:, 1::2, :], op=ALU.mult), reads=[lamt], writes=[lp])
        kb.op("dve", lambda: nc.vector.tensor_reduce(out=ls[:, :], in_=lp[:, :, :], axis=AX.X, op=ALU.add), reads=[lp], writes=[ls])
        kb.op("act", lambda: nc.scalar.activation(out=ls[:, :], in_=ls[:, :], func=AF.Exp), reads=[ls], writes=[ls])
        kb.op("dve", lambda: nc.vector.tensor_tensor(out=neglam[:, :], in0=ls[:, 1:2], in1=ls[:, 0:1], op=ALU.subtract), reads=[ls], writes=[neglam])
        kb.op("dve", lambda: nc.vector.tensor_scalar(out=neglam[:, :], in0=neglam[:, :], scalar1=-lam_init, scalar2=None, op0=ALU.add), reads=[neglam], writes=[neglam])
        sg = kb.sb("sg", [128, 1], F32)
        self.load(sg, sg[:, :], di["b_subln"], di["b_subln"][e, :, :])
        kb.op("dve", lambda: nc.vector.tensor_scalar(out=sg[:, :], in0=sg[:, :], scalar1=(1.0 - lam_init), scalar2=None, op0=ALU.mult), reads=[sg], writes=[sg])
        KTb = [kb.sb(f"KTb{i}", [128, S], BF16) for i in range(2)]
        Qb = [[kb.sb(f"Qb{i}_{mp}", [128, S], BF16) for mp in range(2)] for i in range(2)]
        Vb = [kb.sb(f"Vb{i}", [128, 32, 128], BF16) for i in range(2)]
        for i in range(2):
            for mp in range(2):
                qq = Qb[i][mp]
                kb.op("pool", lambda qq=qq: nc.gpsimd.memset(qq[:, :], 0.0), writes=[qq])
        o1 = kb.sb("o1", [128, 512], F32)
        od = kb.sb("od", [128, 512], F32)
        osq = kb.sb("osq", [128, 512], F32)
        rs = kb.sb("rs", [128, 512], F32)
        obf = [kb.sb(f"obf{i}", [128, 512], BF16) for i in range(2)]
        for h in range(4):
            kt = KTb[h % 2]
            vp = Vb[h % 2]
            self.load(kt, kt[:, :], KBd, KBd[h, :, :])
            for mp in range(2):
                qq = Qb[h % 2][mp]
                self.load(qq, qq[mp * 64:(mp + 1) * 64, :], QB, QB[h, mp * 64:(mp + 1) * 64, :])
            for jj in range(4):
                self.load(vp, vp[:, jj * 8:(jj + 1) * 8, :], VB, VB[jj * 1024:(jj + 1) * 1024, h * 128:(h + 1) * 128].rearrange("(j p) d -> p j d", p=128))
            hb = 8 + h

            def mask_fn2(pt, cs, i, j, hb=hb):
                if i - j in (0, 1):
                    k = hb * 2 + (i - j)
                    kb.op("dve", lambda: nc.vector.tensor_tensor(out=pt[:, cs:cs + 128], in0=pt[:, cs:cs + 128], in1=ct[:, k, :], op=ALU.mult),
                          reads=[pt, ct], writes=[pt])
            for I in range(8):
                for mp in range(2):
                    qq = Qb[h % 2][mp]
                    outs = self.attn_block(I, kt, lambda j, kt=kt: [kt[:, j * 128:(j + 1) * 128]],
                                           qq, lambda a, b, qq=qq: [qq[:, a:b]],
                                           vp, lambda j, vp=vp: [vp[:, j, :]], [128],
                                           lambda i, j, hb=hb: (b31, b31[:, hb:hb + 1]), mask_fn2, onesk)
                    ob = outs[0]
                    if mp == 0:
                        kb.op("act", lambda ob=ob: nc.scalar.copy(out=o1[:, :], in_=ob[:, :]), reads=[ob], writes=[o1])
                    else:
                        kb.op("dve", lambda ob=ob: nc.vector.scalar_tensor_tensor(out=od[:, :], in0=ob[:, :], scalar=neglam[:, 0:1], in1=o1[:, :],
                                                                                  op0=ALU.mult, op1=ALU.add), reads=[ob, neglam, o1], writes=[od])
                kb.op("act", lambda: nc.scalar.activation(out=osq[:, :], in_=od[:, :], func=AF.Square), reads=[od], writes=[osq])
                p5 = self.PS[5]
                self.mm(p5, p5[:, :], ones32, ones32[:, :], osq, osq[:, :], start=True, stop=True)
                kb.op("act", lambda p5=p5: nc.scalar.activation(out=rs[:, :], in_=p5[:, :], func=AF.Sqrt, scale=1.0 / 128, bias=self.eps_col[:, :]),
                      reads=[p5, self.eps_col], writes=[rs])
                kb.op("dve", lambda: nc.vector.reciprocal(out=rs[:, :], in_=rs[:, :]), reads=[rs], writes=[rs])
                of = obf[I % 2]
                kb.op("dve", lambda of=of: nc.vector.scalar_tensor_tensor(out=of[:, :], in0=od[:, :], scalar=sg[:, 0:1], in1=rs[:, :],
                                                                          op0=ALU.mult, op1=ALU.mult), reads=[od, sg, rs], writes=[of])
                self.load(OTB, OTB[h, :, I * 512:(I + 1) * 512], of, of[:, :])
        self.barrier()
        gv, sv, gate = self.load_modvecs(l, 0, "norm_mix")
        otsa = [kb.sb(f"otsa{i}", [64, 8, 128], BF16) for i in range(2)]
        otsb = [kb.sb(f"otsb{i}", [128, 4, 128], BF16) for i in range(2)]

        def ot_loader(ti):
            oa = otsa[ti % 2]
            ob = otsb[ti % 2]
            self.load(oa, oa[:, :, :], OTA, OTA[:, :, ti * 128:(ti + 1) * 128].rearrange("h p t -> p h t"))
            self.load(ob, ob[:, :, :], OTB, OTB[:, :, ti * 128:(ti + 1) * 128].rearrange("h p t -> p h t"))
            return [(oa, oa[:, h, :]) for h in range(8)] + [(ob, ob[:, h, :]) for h in range(4)]
        self.outproj(l, XIN, XOUT, gate, di["even_w_out"], lambda r0, K: di["even_w_out"][e, r0:r0 + K, :],
                     [(64, h * 64) for h in range(8)] + [(128, 512 + h * 128) for h in range(4)], ot_loader)

    def phase_mixer(self, l, XIN, XOUT):
        self.convert_uv(l)
        if l % 2 == 0:
            return self.phase_even(l, XIN, XOUT)
        nc, kb, di = self.nc, self.kb, self.di
        o = l // 2
        gv, sv, gate = self.load_modvecs(l, 0, "norm_mix")
        w16 = kb.sb("w16", [128, 8, 3088], BF16)
        self.load_w16(w16, di["odd_w_in"], lambda kc: di["odd_w_in"][o, kc * 128:(kc + 1) * 128, :], 3088)
        self.barrier(reset=False)
        hT = kb.sb("hT", [128, 8, 256], BF16)
        QC, KC, VC, OT = self.QC, self.KC, self.VC, self.OT
        LF = kb.sb("LF", [16, S], F32)
        bfc = kb.sb("bfc", [16, 1], F32)
        self.load(bfc, bfc[:, :], di["odd_b_forget"], di["odd_b_forget"][o, :, :])
        qst = [kb.sb(f"qst{i}", [128, 8, 256], BF16) for i in range(2)]
        kst = [kb.sb(f"kst{i}", [128, 8, 256], BF16) for i in range(2)]
        vst = [kb.sb(f"vst{i}", [128, 1024], BF16) for i in range(2)]
        ft = kb.sb("ft", [16, 256], F32)
        fm, tm = [], []
        for m in range(8):
            def fq(pb, blk, m=m):
                q = qst[blk % 2]
                kb.op("act", lambda: nc.scalar.activation(out=q[:, m, :], in_=pb[:, 0:256], func=AF.Copy, scale=0.125),
                      reads=[pb], writes=[q])
                if m == 7:
                    self.load(QC, QC[:, :, blk * 256:(blk + 1) * 256].rearrange("m p t -> p m t"), q, q[:, :, :])
            fm.append((m * 128, 128, fq))
        for m in range(8):
            def fk(pb, blk, m=m):
                k = kst[blk % 2]
                kb.op("dve", lambda: nc.vector.tensor_copy(out=k[:, m, :], in_=pb[:, 0:256]), reads=[pb], writes=[k])
                if m == 7:
                    self.load(KC, KC[:, :, blk * 256:(blk + 1) * 256].rearrange("m p t -> p m t"), k, k[:, :, :])
            fm.append((1024 + m * 128, 128, fk))

        def ff(pb, blk):
            kb.op("dve", lambda: nc.vector.tensor_scalar(out=ft[:, :], in0=pb[0:16, 0:256], scalar1=bfc[:, :], scalar2=-1.0,
                                                         op0=ALU.add, op1=ALU.mult), reads=[pb, bfc], writes=[ft])
            kb.op("act", lambda: nc.scalar.activation(out=ft[:, :], in_=ft[:, :], func=AF.Exp), reads=[ft], writes=[ft])
            kb.op("act", lambda: nc.scalar.activation(out=ft[:, :], in_=ft[:, :], func=AF.Ln, bias=1.0), reads=[ft], writes=[ft])
            kb.op("dve", lambda: nc.vector.tensor_scalar(out=LF[:, blk * 256:(blk + 1) * 256], in0=ft[:, :], scalar1=-1.0,
                                                         scalar2=None, op0=ALU.mult), reads=[ft], writes=[LF])
        fm.append((3072, 16, ff))
        for half in range(2):
            def fv(pb, ti, half=half):
                v = vst[ti % 2]
                if half == 0:
                    kb.op("act", lambda: nc.scalar.copy(out=v[:, 0:512], in_=pb[:, :]), reads=[pb], writes=[v])
                else:
                    kb.op("dve", lambda: nc.vector.tensor_copy(out=v[:, 512:1024], in_=pb[:, :]), reads=[pb], writes=[v])
                    self.load(VC, VC[ti * 128:(ti + 1) * 128, :], v, v[:, :])
            tm.append((2048 + half * 512, 512, fv))
        self.inproj_blocks(XIN, gv, sv, w16, fm, tm, hT)
        onesb = kb.sb("ones16", [16, S], BF16)
        cum = kb.sb("cum", [16, S], F32)
        kb.op("dve", lambda: nc.vector.memset(onesb[:, :], 1.0), writes=[onesb])
        kb.op("dve", lambda: nc.vector.tensor_tensor_scan(out=cum[:, :], data0=onesb[:, :], data1=LF[:, :], initial=0.0,
                                                          op0=ALU.mult, op1=ALU.add), reads=[onesb, LF], writes=[cum])
        cqr = kb.sb("cqr", [16, 8, 512], BF16)
        kb.op("dve", lambda: nc.vector.tensor_tensor(
            out=cqr[:, :, :], in0=cum[:, :].rearrange("p (j t) -> p j t", t=512),
            in1=cum[:, :].rearrange("p (j t) -> p j t", t=512)[:, :, 511:512].to_broadcast([16, 8, 512]), op=ALU.subtract),
            reads=[cum], writes=[cqr])
        self.load(self.CQ, self.CQ[:, :], cqr, cqr[:, :, :].rearrange("p j t -> p (j t)"))
        cumT = kb.sb("cumT", [128, 32, 16], F32, persist=True)
        refbc = kb.sb("refbc", [128, 32, 16], F32, persist=True)
        pc = self.PS[0]
        for j in range(32):
            kb.op("pe", lambda j=j: nc.tensor.transpose(pc[:, j * 16:(j + 1) * 16], cum[:, j * 128:(j + 1) * 128], self.ident[0:16, 0:16]),
                  reads=[cum, self.ident], writes=[pc])
        kb.op("dve", lambda: nc.vector.tensor_copy(out=cumT[:, :, :], in_=pc[:, :].rearrange("p (j h) -> p j h", h=16)),
              reads=[pc], writes=[cumT])
        sel = kb.sb("sel127", [128, 128], F32)
        self.load(sel, sel[:, :], di["sel127"], di["sel127"][:, :])
        pr = self.PS[1]
        self.mm(pr, pr[:, :], sel, sel[:, :], cumT, cumT[:, :, :].rearrange("p j h -> p (j h)"), start=True, stop=True)
        kb.op("dve", lambda: nc.vector.tensor_copy(out=refbc[:, :, :], in_=pr[:, :].rearrange("p (j h) -> p j h", h=16)),
              reads=[pr], writes=[refbc])
        self.barrier()
        tri = kb.sb("tri", [128, 128], BF16)
        tri32 = Buf(kb.sb("xo", [128, D], F32)[:, 0:128], "tri32")
        self.load(tri32, tri32[:, :], di["tri"], di["tri"][:, :])
        kb.op("dve", lambda: nc.vector.tensor_copy(out=tri[:, :], in_=tri32[:, :]), reads=[tri32], writes=[tri])
        onesk = kb.sb("onesk", [128, 128], BF16)
        kb.op("dve", lambda: nc.vector.memset(onesk[:, :], 1.0), writes=[onesk])
        cq = kb.sb("cq", [16, S], BF16)
        self.load(cq, cq[:, :], self.CQ, self.CQ[:, :])
        sel16 = kb.sb("sel16", [16, 16, 128], BF16)
        kb.op("dve", lambda: nc.vector.tensor_copy(out=sel16[:, :, :], in_=self.ident[0:16, 0:16].unsqueeze(2).to_broadcast([16, 16, 128])),
              reads=[self.ident], writes=[sel16])
        KT = [kb.sb(f"KTp{i}", [128, S], BF16) for i in range(2)]
        Qp = [[kb.sb(f"Qp{i}_{hh}", [128, S], BF16) for hh in range(2)] for i in range(2)]
        Vp = [kb.sb(f"Vp{i}", [128, 32, 128], BF16) for i in range(2)]
        Bt = [kb.sb(f"Bt{i}", [128, 8, 32], F32) for i in range(2)]
        for i in range(2):
            for hh in range(2):
                q = Qp[i][hh]
                kb.op("pool", lambda q=q: nc.gpsimd.memset(q[:, :], 0.0), writes=[q])
        self._attn_n = 0
        self._attn_par = 0
        for m in range(8):
            kt = KT[m % 2]
            vp = Vp[m % 2]
            self.load(kt, kt[:, :], KC, KC[m, :, :])
            for hh in range(2):
                q = Qp[m % 2][hh]
                self.load(q, q[hh * 64:(hh + 1) * 64, :], QC, QC[m, hh * 64:(hh + 1) * 64, :])
            for jj in range(4):
                self.load(vp, vp[:, jj * 8:(jj + 1) * 8, :],
                          VC, VC[jj * 1024:(jj + 1) * 1024, m * 128:(m + 1) * 128].rearrange("(j p) d -> p j d", p=128))
            for hh in range(2):
                h = m * 2 + hh
                bt = Bt[h % 2]
                kb.op("dve", lambda bt=bt, h=h: nc.vector.tensor_tensor(
                    out=bt[:, :, :], in0=refbc[:, 3::4, h].unsqueeze(2).to_broadcast([128, 8, 32]),
                    in1=cumT[:, :, h].unsqueeze(1).to_broadcast([128, 8, 32]), op=ALU.subtract),
                    reads=[refbc, cumT], writes=[bt])
                q = Qp[m % 2][hh]

                def mask_fn(pt, cs, i, j):
                    if i == j:
                        kb.op("pool", lambda: nc.gpsimd.tensor_tensor(out=pt[:, cs:cs + 128], in0=pt[:, cs:cs + 128],
                                                                      in1=tri[:, :], op=ALU.mult), reads=[pt, tri], writes=[pt])
                for I in range(8):
                    outs = self.attn_block(I, kt, lambda j, kt=kt: [kt[:, j * 128:(j + 1) * 128]],
                                           q, lambda a, b, q=q: [q[:, a:b]],
                                           vp, lambda j, vp=vp, hh=hh: [vp[:, j, hh * 64:(hh + 1) * 64]], [64],
                                           lambda i, j, bt=bt: (bt, bt[:, i, j:j + 1]), mask_fn, onesk,
                                           extra=(sel16, sel16[:, h, :], cq, lambda a, b: cq[:, a:b]), clamp_diag=True)
                    self.load(OT, OT[h, :, I * 512:(I + 1) * 512], outs[0], outs[0][0:64, :])
        self.barrier()
        gv, sv, gate = self.load_modvecs(l, 0, "norm_mix")
        ots = [kb.sb(f"ots{i}", [64, 16, 128], BF16) for i in range(2)]

        def ot_loader(ti):
            ob = ots[ti % 2]
            self.load(ob, ob[:, :, :], OT, OT[:, :, ti * 128:(ti + 1) * 128].rearrange("h p t -> p h t"))
            return [(ob, ob[:, h, :]) for h in range(16)]
        self.outproj(l, XIN, XOUT, gate, di["odd_w_out"], lambda r0, K: di["odd_w_out"][o, r0:r0 + K, :],
                     [(64, h * 64) for h in range(16)], ot_loader)

    def phase_peer(self, l, XIN, XOUT):
        nc, kb, di = self.nc, self.kb, self.di
        gv, sv, gate = self.load_modvecs(l, 1, "norm_ffn")
        w16 = kb.sb("w16p", [128, 8, 2048], BF16)
        self.load_w16(w16, di["peer_w_q"], lambda kc: di["peer_w_q"][l, kc * 128:(kc + 1) * 128, :], 2048, stage="uvrow")
        self.barrier(reset=False)
        skT = kb.sb("skT", [128, 2, 128], F32)
        for p in range(2):
            self.load(skT, skT[:, p, :], di["peer_skT"], di["peer_skT"][l, p, :, :])
        iota16 = kb.sb("iota16", [128, 16], F32)
        self.load(iota16, iota16[:, :], di["iota16"], di["iota16"][:, :])
        hT = kb.sb("hT", [128, 8, 256], BF16)
        qT = kb.sb("qT", [128, 16, 256], F32)
        xts = [kb.sb(f"xt{i}", [128, D], F32) for i in range(2)]
        hts = [kb.sb(f"htok{i}", [128, D], F32) for i in range(2)]
        h16 = [kb.sb(f"h16_{i}", [128, D], BF16) for i in range(2)]
        ssb = kb.sb("ssb", [128, 16, 128], F32)
        tv = kb.sb("tv", [128, 16, 16], F32)
        tiu = kb.sb("tiu", [128, 16, 16], U32)
        tif = kb.sb("tif", [128, 16, 16], F32)
        cand = kb.sb("cand", [128, 8, 16, 16], F32)
        bv = kb.sb("bv", [128, 8, 16], F32)
        bpu = kb.sb("bpu", [128, 8, 16], U32)
        bi_u = kb.sb("bi_u", [128, 8, 16], U32)
        bj_u = kb.sb("bj_u", [128, 8, 16], U32)
        bi_f = kb.sb("bi_f", [128, 8, 16], F32)
        bj_f = kb.sb("bj_f", [128, 8, 16], F32)
        eq = kb.sb("eq", [128, 8, 16, 16], F32)
        tva = [Buf(tv[:, g, 0:8], f"tva{g}") for g in range(16)]
        tvb = [Buf(tv[:, g, 8:16], f"tvb{g}") for g in range(16)]
        tia = [Buf(tiu[:, g, 0:8], f"tia{g}") for g in range(16)]
        tib = [Buf(tiu[:, g, 8:16], f"tib{g}") for g in range(16)]
        s2g = [kb.sb(f"s2g{g}", [128, 128], F32) for g in range(16)]
        s2h = [kb.sb(f"s2h{g}", [128, 256], F32) for g in range(4)]
        bva = [Buf(bv[:, h, 0:8], f"bva{h}") for h in range(8)]
        bvb = [Buf(bv[:, h, 8:16], f"bvb{h}") for h in range(8)]
        bpa = [Buf(bpu[:, h, 0:8], f"bpa{h}") for h in range(8)]
        bpb = [Buf(bpu[:, h, 8:16], f"bpb{h}") for h in range(8)]
        n0 = kb.sb("n0", [128, 8, 16], F32)
        n1 = kb.sb("n1", [128, 8, 16], F32)
        ef = kb.sb("ef", [128, 128], F32)
        eidx = [kb.sb(f"eidx{i}", [128, 128], U32) for i in range(2)]
        gsum = kb.sb("gsum", [128, 8], F32)
        gat = kb.sb("gat", [128, 8, 16], F32)
        actv = kb.sb("actv", [128, 128], F32)
        t1 = kb.sb("t1", [128, 128], F32)
        xg = kb.sb("xg", [128, 128], F32)
        wgt = [kb.sb(f"wgt{i}", [128, 128], F32) for i in range(2)]
        NG = 12
        ug = [kb.sb(f"uvrow{i}", [128, 2 * D], BF16) for i in range(NG)]
        vs = [kb.sb(f"vs{i}", [128, D], BF16) for i in range(2)]
        junk2 = kb.sb("junk2", [128, D], BF16)
        xo = kb.sb("xo", [128, D], F32)
        self.convert_uv(l)
        UV = self.UV16L[l]
        gi = 0
        for blk in range(S // 256):
            for tt in range(2):
                ti = blk * 2 + tt
                xt = xts[tt]
                self.load(xt, xt[:, :], XIN, XIN[ti * 128:(ti + 1) * 128, :])
                self.norm_tile(xt, gv, sv, hts[tt], h16[tt % 2])
                self.transpose_tile(h16[tt % 2], hT, tt * 128)
            for g in range(16):
                pb = self.PS[g % 2]
                for kc in range(8):
                    self.mm(pb, pb[:, 0:256], w16, w16[:, kc, g * 128:(g + 1) * 128], hT, hT[:, kc, :],
                            start=(kc == 0), stop=(kc == 7))
                if g % 2 == 0:
                    kb.op("act", lambda pb=pb, g=g: nc.scalar.copy(out=qT[:, g, :], in_=pb[:, 0:256]), reads=[pb], writes=[qT])
                else:
                    kb.op("dve", lambda pb=pb, g=g: nc.vector.tensor_copy(out=qT[:, g, :], in_=pb[:, 0:256]), reads=[pb], writes=[qT])
            for tt in range(2):
                ti = blk * 2 + tt
                xt = xts[tt]
                htok = hts[tt]
                for g in range(16):
                    pb = self.PS[2 + g // 4]
                    self.mm(pb, pb[:, (g % 4) * 128:(g % 4 + 1) * 128], qT, qT[:, g, tt * 128:(tt + 1) * 128],
                            skT, skT[:, g % 2, :], start=True, stop=True)
                for q4 in range(4):
                    pb = self.PS[2 + q4]
                    kb.op("act", lambda pb=pb, q4=q4: nc.scalar.copy(
                        out=ssb[:, q4 * 4:(q4 + 1) * 4, :], in_=pb[:, :].rearrange("p (g n) -> p g n", g=4)),
                        reads=[pb], writes=[ssb])
                for g in range(16):
                    kb.op("dve", lambda g=g: nc.vector.max(out=tv[:, g, 0:8], in_=ssb[:, g, :]), reads=[ssb], writes=[tva[g]])
                for g in range(16):
                    kb.op("dve", lambda g=g: nc.vector.max_index(out=tiu[:, g, 0:8], in_max=tv[:, g, 0:8], in_values=ssb[:, g, :]),
                          reads=[ssb, tva[g]], writes=[tia[g]])
                for g in range(16):
                    kb.op("dve", lambda g=g: nc.vector.match_replace(out=s2g[g][:, :], in_to_replace=tv[:, g, 0:8],
                                                                     in_values=ssb[:, g, :], imm_value=-1e30),
                          reads=[ssb, tva[g]], writes=[s2g[g]])
                for g in range(16):
                    kb.op("dve", lambda g=g: nc.vector.max(out=tv[:, g, 8:16], in_=s2g[g][:, :]), reads=[s2g[g]], writes=[tvb[g]])
                for g in range(16):
                    kb.op("dve", lambda g=g: nc.vector.max_index(out=tiu[:, g, 8:16], in_max=tv[:, g, 8:16], in_values=s2g[g][:, :]),
                          reads=[s2g[g], tvb[g]], writes=[tib[g]])
                kb.op("dve", lambda: nc.vector.tensor_copy(out=tif[:, :, :], in_=tiu[:, :, :]), reads=tia + tib, writes=[tif])
                kb.op("dve", lambda: nc.vector.tensor_tensor(
                    out=cand[:, :, :, :], in0=tv[:, 0::2, :].unsqueeze(3).to_broadcast([128, 8, 16, 16]),
                    in1=tv[:, 1::2, :].unsqueeze(2).to_broadcast([128, 8, 16, 16]), op=ALU.add),
                    reads=tva + tvb, writes=[cand])
                for hb in range(2):
                    hs = range(hb * 4, hb * 4 + 4)
                    cfs = {h: cand[:, h, :, :].rearrange("p a b -> p (a b)") for h in hs}
                    for h in hs:
                        kb.op("dve", lambda h=h, cf=cfs[h]: nc.vector.max(out=bv[:, h, 0:8], in_=cf), reads=[cand], writes=[bva[h]])
                    for h in hs:
                        kb.op("dve", lambda h=h, cf=cfs[h]: nc.vector.max_index(out=bpu[:, h, 0:8], in_max=bv[:, h, 0:8], in_values=cf),
                              reads=[cand, bva[h]], writes=[bpa[h]])
                    for h in hs:
                        kb.op("dve", lambda h=h, cf=cfs[h]: nc.vector.match_replace(out=s2h[h % 4][:, :], in_to_replace=bv[:, h, 0:8],
                                                                         in_values=cf, imm_value=-1e30),
                              reads=[cand, bva[h]], writes=[s2h[h % 4]])
                    for h in hs:
                        kb.op("dve", lambda h=h: nc.vector.max(out=bv[:, h, 8:16], in_=s2h[h % 4][:, :]), reads=[s2h[h % 4]], writes=[bvb[h]])
                    for h in hs:
                        kb.op("dve", lambda h=h: nc.vector.max_index(out=bpu[:, h, 8:16], in_max=bv[:, h, 8:16], in_values=s2h[h % 4][:, :]),
                              reads=[s2h[h % 4], bvb[h]], writes=[bpb[h]])
                bvall = bva + bvb
                bpall = bpa + bpb
                kb.op("dve", lambda: nc.vector.tensor_scalar(out=bi_u[:, :, :], in0=bpu[:, :, :], scalar1=4, scalar2=None,
                                                             op0=ALU.logical_shift_right), reads=bpall, writes=[bi_u])
                kb.op("dve", lambda: nc.vector.tensor_scalar(out=bj_u[:, :, :], in0=bpu[:, :, :], scalar1=15, scalar2=None,
                                                             op0=ALU.bitwise_and), reads=bpall, writes=[bj_u])
                kb.op("dve", lambda: nc.vector.tensor_copy(out=bi_f[:, :, :], in_=bi_u[:, :, :]), reads=[bi_u], writes=[bi_f])
                kb.op("dve", lambda: nc.vector.tensor_copy(out=bj_f[:, :, :], in_=bj_u[:, :, :]), reads=[bj_u], writes=[bj_f])
                io_b = iota16[:, :].unsqueeze(1).unsqueeze(1).to_broadcast([128, 8, 16, 16])
                for (bf, par, nn) in ((bi_f, 0, n0), (bj_f, 1, n1)):
                    kb.op("dve", lambda bf=bf: nc.vector.tensor_tensor(
                        out=eq[:, :, :, :], in0=bf[:, :, :].unsqueeze(3).to_broadcast([128, 8, 16, 16]), in1=io_b,
                        op=ALU.is_equal), reads=[bf, iota16], writes=[eq])
                    kb.op("dve", lambda par=par: nc.vector.tensor_tensor(
                        out=eq[:, :, :, :], in0=eq[:, :, :, :],
                        in1=tif[:, par::2, :].unsqueeze(2).to_broadcast([128, 8, 16, 16]), op=ALU.mult),
                        reads=[eq, tif], writes=[eq])
                    kb.op("dve", lambda nn=nn: nc.vector.tensor_reduce(out=nn[:, :, :], in_=eq[:, :, :, :], axis=AX.X, op=ALU.add),
                          reads=[eq], writes=[nn])
                kb.op("dve", lambda: nc.vector.scalar_tensor_tensor(
                    out=ef[:, :], in0=n0[:, :, :].rearrange("p a b -> p (a b)"), scalar=128.0,
                    in1=n1[:, :, :].rearrange("p a b -> p (a b)"), op0=ALU.mult, op1=ALU.add),
                    reads=[n0, n1], writes=[ef])
                if l > 0:
                    kb.op("dve", lambda: nc.vector.tensor_scalar(out=ef[:, :], in0=ef[:, :], scalar1=float(l * 16384), scalar2=None,
                                                                 op0=ALU.add), reads=[ef], writes=[ef])
                ei = eidx[ti % 2]
                kb.op("dve", lambda ei=ei: nc.vector.tensor_copy(out=ei[:, :], in_=ef[:, :]), reads=[ef], writes=[ei])
                kb.op("dve", lambda: nc.vector.tensor_tensor(
                    out=gat[:, :, :], in0=bv[:, :, :], in1=bv[:, :, 0:1].to_broadcast([128, 8, 16]), op=ALU.subtract),
                    reads=bvall, writes=[gat])
                kb.op("act", lambda: nc.scalar.activation(out=gat[:, :, :], in_=gat[:, :, :], func=AF.Exp),
                      reads=[gat], writes=[gat])
                kb.op("dve", lambda: nc.vector.tensor_reduce(out=gsum[:, :], in_=gat[:, :, :], axis=AX.X, op=ALU.add),
                      reads=[gat], writes=[gsum])
                kb.op("dve", lambda: nc.vector.reciprocal(out=gsum[:, :], in_=gsum[:, :]), reads=[gsum], writes=[gsum])
                kb.op("dve", lambda: nc.vector.tensor_tensor(
                    out=gat[:, :, :], in0=gat[:, :, :], in1=gsum[:, :].unsqueeze(2).to_broadcast([128, 8, 16]), op=ALU.mult),
                    reads=[gat, gsum], writes=[gat])
                C1 = 0.044715
                C2 = 2.0 * math.sqrt(2.0 / math.pi)
                wg = wgt[ti % 2]
                h16t = h16[tt % 2]
                gatf = gat[:, :, :].rearrange("p a b -> p (a b)")
                po0, po1 = self.PS[0], self.PS[1]
                def tail(g4, rows):
                    c_ = slice(g4 * 4, g4 * 4 + 4)
                    kb.op("dve", lambda: nc.vector.tensor_tensor(out=wg[:, c_], in0=t1[:, c_], in1=xg[:, c_], op=ALU.mult),
                          reads=[t1, xg], writes=[wg])
                    for k in range(4):
                        sl = g4 * 4 + k
                        u = rows[k]
                        vsb = vs[sl % 2]
                        kb.op("act", lambda u=u, vsb=vsb, sl=sl: nc.scalar.activation(
                            out=vsb[:, :], in_=u[:, D:2 * D], func=AF.Copy, scale=wg[:, sl:sl + 1]),
                            reads=[u, wg], writes=[vsb])
                        self.mm(po0, po0[:, :], self.identb, self.identb[:, :], vsb, vsb[:, 0:512], start=(sl == 0), stop=(sl == 127))
                        self.mm(po1, po1[:, :], self.identb, self.identb[:, :], vsb, vsb[:, 512:1024], start=(sl == 0), stop=(sl == 127))

                pending = None
                for g4 in range(32):
                    rows = []
                    for k in range(4):
                        sl = g4 * 4 + k
                        u = ug[gi % NG]
                        gi += 1
                        rows.append(u)
                        kb.dma_op("pool", lambda u=u, ei=ei, sl=sl: nc.gpsimd.indirect_dma_start(
                            out=u[:, :], out_offset=None, in_=UV[:, :],
                            in_offset=bass.IndirectOffsetOnAxis(ap=ei[:, sl:sl + 1], axis=0)),
                            reads=[UV, ei], writes=[u])
                        kb.op("dve", lambda u=u, sl=sl, h16t=h16t: nc.vector.scalar_tensor_tensor(
                            out=junk2[:, :], in0=u[:, 0:D], scalar=1.0, in1=h16t[:, :], op0=ALU.mult, op1=ALU.mult,
                            accum_out=actv[:, sl:sl + 1]), reads=[u, h16t], writes=[junk2, actv])
                    c_ = slice(g4 * 4, g4 * 4 + 4)
                    kb.op("dve", lambda c_=c_: nc.vector.tensor_tensor(out=t1[:, c_], in0=actv[:, c_], in1=actv[:, c_], op=ALU.mult),
                          reads=[actv], writes=[t1])
                    kb.op("dve", lambda c_=c_: nc.vector.tensor_scalar(out=t1[:, c_], in0=t1[:, c_], scalar1=C1, scalar2=1.0,
                                                                       op0=ALU.mult, op1=ALU.add), reads=[t1], writes=[t1])
                    kb.op("dve", lambda c_=c_: nc.vector.tensor_tensor(out=t1[:, c_], in0=t1[:, c_], in1=actv[:, c_], op=ALU.mult),
                          reads=[t1, actv], writes=[t1])
                    kb.op("dve", lambda c_=c_: nc.vector.tensor_tensor(out=xg[:, c_], in0=actv[:, c_], in1=gatf[:, c_], op=ALU.mult),
                          reads=[actv, gat], writes=[xg])
                    kb.op("act", lambda c_=c_: nc.scalar.activation(out=t1[:, c_], in_=t1[:, c_], func=AF.Sigmoid, scale=C2),
                          reads=[t1], writes=[t1])
                    if pending is not None:
                        tail(*pending)
                    pending = (g4, rows)
                tail(*pending)
                kb.op("dve", lambda: nc.vector.tensor_tensor(out=xo[:, 0:512], in0=po0[:, :], in1=gate[:, 0:512], op=ALU.mult),
                      reads=[po0, gate], writes=[xo])
                kb.op("dve", lambda: nc.vector.tensor_tensor(out=xo[:, 512:1024], in0=po1[:, :], in1=gate[:, 512:1024], op=ALU.mult),
                      reads=[po1, gate], writes=[xo])
                kb.op("dve", lambda xt=xt: nc.vector.tensor_tensor(out=xo[:, :], in0=xo[:, :], in1=xt[:, :], op=ALU.add),
                      reads=[xo, xt], writes=[xo])
                self.load(XOUT, XOUT[ti * 128:(ti + 1) * 128, :], xo, xo[:, :])

    def phase_final(self, XIN, XOUT, do_norm):
        nc, kb, di = self.nc, self.kb, self.di
        nv = kb.sb("gvec", [128, D], F32)
        zv = kb.sb("shvec", [128, D], F32)
        if do_norm:
            self.load(nv, nv[:, :], di["norm_final"], di["norm_final"][:, :].partition_broadcast(128))
            kb.op("dve", lambda: nc.vector.memset(zv[:, :], 0.0), writes=[zv])
        xts = [kb.sb(f"xt{i}", [128, D], F32) for i in range(2)]
        hts = [kb.sb(f"htok{i}", [128, D], F32) for i in range(2)]
        for ti in range(NT):
            xt = xts[ti % 2]
            self.load(xt, xt[:, :], XIN, XIN[ti * 128:(ti + 1) * 128, :])
            if do_norm:
                self.norm_tile(xt, nv, zv, hts[ti % 2], None)
                self.load(XOUT, XOUT[ti * 128:(ti + 1) * 128, :], hts[ti % 2], hts[ti % 2][:, :])
            else:
                self.load(XOUT, XOUT[ti * 128:(ti + 1) * 128, :], xt, xt[:, :])

    def build(self):
        self.phase_mods()
        self.barrier()
        cur = self.di["x"]
        pp = [self.XA, self.XB]
        n = 0
        for l in self.layers:
            if not self.dbg_peer_only:
                nxt = pp[n % 2]; n += 1
                self.phase_mixer(l, cur, nxt)
                self.barrier()
                cur = nxt
            nxt = pp[n % 2]; n += 1
            self.phase_peer(l, cur, nxt)
            self.barrier()
            cur = nxt
        self.phase_final(cur, self.out, self.final_norm)
        self.kb.finish([self.out])
        self.kb.emit()
        return self.nc


def _bucket_idx(dist):
    max_exact = 16
    d = np.maximum(dist, 0)
    df = np.maximum(d, 1).astype(np.float32)
    large = max_exact + (np.log(df / max_exact) / np.float32(math.log(128 / max_exact)) * (32 - max_exact)).astype(np.int32)
    large = np.minimum(large, 31)
    return np.where(d < max_exact, d, large)


def _bias_tiles(rb):
    s_ = np.arange(128)[:, None]
    t_ = np.arange(128)[None, :]
    out = np.zeros((128, 24, 128), dtype=np.float32)
    for dl in range(2):
        idx = _bucket_idx(128 * dl + t_ - s_)
        for h in range(12):
            out[:, h * 2 + dl, :] = rb[idx, h]
    return out


class _IM(dict):
    def __setitem__(self, k, v):
        super().__setitem__(k if k.startswith("i_") else "i_" + k, v)


def host_shared(inputs):
    f = np.float32
    u = np.asarray(inputs["peer_u"], dtype=f).reshape(DEPTH * 16384, D)
    v = np.asarray(inputs["peer_v"], dtype=f).reshape(DEPTH * 16384, D)
    return {"peer_uv": np.ascontiguousarray(np.concatenate([u, v], axis=1))}


def host_inputs(inputs, b, shared=None):
    f = np.float32
    if shared is None:
        shared = host_shared(inputs)
    m = _IM()
    m["x"] = np.ascontiguousarray(inputs["x"][b], dtype=f)
    m["ccol"] = np.ascontiguousarray(inputs["c"][b].reshape(8, 128).T, dtype=f)
    m["ident"] = np.eye(128, dtype=f)
    m["ada_w"] = np.asarray(inputs["ada_w"], dtype=f)
    m["ada_b"] = np.asarray(inputs["ada_b"], dtype=f).reshape(DEPTH, 1, 6 * D)
    m["norm_mix"] = np.asarray(inputs["norm_mix"], dtype=f).reshape(DEPTH, 1, D)
    m["norm_ffn"] = np.asarray(inputs["norm_ffn"], dtype=f).reshape(DEPTH, 1, D)
    m["norm_final"] = np.asarray(inputs["norm_final"], dtype=f).reshape(1, D)
    m["peer_w_q"] = np.asarray(inputs["peer_w_q"], dtype=f)
    m["peer_skT"] = np.ascontiguousarray(np.transpose(np.asarray(inputs["peer_sub_keys"], dtype=f), (0, 1, 3, 2)))
    m["peer_uv"] = shared["peer_uv"]
    m["odd_w_in"] = np.asarray(inputs["odd_w_in"], dtype=f)
    m["odd_b_forget"] = np.asarray(inputs["odd_b_forget"], dtype=f).reshape(2, 16, 1)
    m["odd_w_out"] = np.asarray(inputs["odd_w_out"], dtype=f)
    m["even_w_in"] = np.asarray(inputs["even_w_in"], dtype=f)
    m["even_w_out"] = np.asarray(inputs["even_w_out"], dtype=f)
    m["a_kv_norm"] = np.asarray(inputs["a_kv_norm"], dtype=f).reshape(2, 1, 256)
    m["ukT"] = np.ascontiguousarray(np.transpose(np.asarray(inputs["a_w_uk"], dtype=f), (0, 2, 3, 1)))
    m["a_w_uv"] = np.asarray(inputs["a_w_uv"], dtype=f).reshape(2, 256, 512)
    m["b_lambda"] = np.asarray(inputs["b_lambda"], dtype=f)
    m["b_subln"] = np.asarray(inputs["b_subln"], dtype=f).reshape(2, 128, 1)
    rb = np.asarray(inputs["rel_bias"], dtype=f)
    m["b31rep"] = np.ascontiguousarray(np.broadcast_to(rb[31:32, :], (128, 12)))
    m["biasT"] = _bias_tiles(rb)
    m["negtri"] = np.ascontiguousarray((np.tril(np.ones((128, 128), dtype=f), -1).T * -1e30).astype(f))
    sel = np.zeros((128, 128), dtype=f); sel[127, :] = 1.0
    m["sel127"] = sel
    m["tri"] = np.triu(np.ones((128, 128), dtype=f))
    m["iota16"] = np.ascontiguousarray(np.broadcast_to(np.arange(16, dtype=f)[None, :], (128, 16)))
    return m


def kernel(**inputs):
    prog = Prog(layers=list(range(DEPTH)), final_norm=True)
    nc = prog.build()
    ncores = 4
    shared = host_shared(inputs)
    in_maps = [host_inputs(inputs, b, shared) for b in range(ncores)]
    res = run_bass_kernel_spmd(nc, in_maps, core_ids=list(range(ncores)))
    return np.stack([np.asarray(r["out"], dtype=np.float32) for r in res.results], axis=0)
```

```python
import math
import numpy as np
import concourse.bass as bass
import concourse.mybir as mybir
from concourse.bass_utils import run_bass_kernel_spmd
from contextlib import ExitStack

F32 = mybir.dt.float32
BF16 = mybir.dt.bfloat16
I32 = mybir.dt.int32
U32 = mybir.dt.uint32
AF = mybir.ActivationFunctionType
ALU = mybir.AluOpType
AX = mybir.AxisListType

EPOCH = 30000
NDMASEM = 12

S = 4096
D = 1024
NT = S // 128
DEPTH = 4
EPS = 1e-6


class Buf:
    __slots__ = ("t", "lw", "rd", "name")

    def __init__(self, t, name=""):
        self.t = t
        self.lw = None
        self.rd = {}
        self.name = name

    def __getitem__(self, idx):
        return self.t[idx]


class KB:
    def __init__(self, nc):
        self.nc = nc
        self.es = ExitStack()
        self.eng = {"pe": nc.tensor, "act": nc.scalar, "dve": nc.vector,
                    "pool": nc.gpsimd, "sp": nc.sync}
        self.sems = {}
        self.cur = {}
        self.seen = {e: {} for e in self.eng}
        self.nsem = 0
        self.prog = {e: [] for e in self.eng}
        for e in self.eng:
            self._new_epoch(e)
        self.dma = {}
        for e in ("sp", "pool"):
            pool = []
            for i in range(NDMASEM):
                k = f"d_{e}_{i}"
                self.sems[k] = nc.alloc_semaphore(name=k)
                pool.append([k, 0])
            self.dma[e] = [pool, 0]
        self.ninstr = {e: 0 for e in self.eng}
        self.cache = {}
        self.pcache = {}
        self.arena = None
        self.bump = 0

    def emit(self):
        nc = self.nc
        with nc.Block() as block:
            for e, dec in (("pe", block.tensor), ("act", block.scalar), ("dve", block.vector),
                           ("pool", block.gpsimd), ("sp", block.sync)):
                prog = self.prog[e]
                eng = self.eng[e]

                def body(_x, prog=prog, eng=eng):
                    for it in prog:
                        if it[0] == "w":
                            eng.wait_ge(it[1], it[2])
                        else:
                            ins = it[1]()
                            ins.then_inc(it[2], it[3])
                dec(body)

    def _new_epoch(self, e):
        k = f"s_{e}_{self.nsem}"
        self.nsem += 1
        self.sems[k] = self.nc.alloc_semaphore(name=k)
        self.cur[e] = [k, 0]

    ARENA = 50000

    def sb(self, name, shape, dt, persist=False):
        if name in self.cache:
            return self.cache[name]
        if name in self.pcache:
            return self.pcache[name]
        if persist:
            t = self.es.enter_context(self.nc.sbuf_tensor(name, list(shape), dt))
            b = Buf(t, name)
            self.pcache[name] = b
            return b
        if self.arena is None:
            self.arena = self.es.enter_context(self.nc.sbuf_tensor("arena", [128, self.ARENA], F32))
        isz = 4 if dt in (F32, U32, I32) else 2
        nel = int(np.prod(shape[1:]))
        nw = (nel * isz + 31) // 32 * 8
        off = self.bump
        self.bump += nw
        assert self.bump <= self.ARENA, f"arena overflow at {name}: {self.bump * 4}"
        ap = self.arena[:, off:off + nw]
        if dt != F32:
            ap = ap.bitcast(dt)
        ap = ap[0:shape[0], 0:nel]
        if len(shape) == 3:
            ap = ap.rearrange("p (a b) -> p a b", a=shape[1])
        elif len(shape) == 4:
            ap = ap.rearrange("p (a b c) -> p a b c", a=shape[1], b=shape[2])
        b = Buf(ap, name)
        self.cache[name] = b
        return b

    def phase_reset(self):
        self.cache = {}
        self.bump = 0

    def ps(self, name, shape, dt=F32):
        if name in self.cache:
            return self.cache[name]
        t = self.es.enter_context(self.nc.psum_tensor(name, list(shape), dt))
        b = Buf(t, name)
        self.cache[name] = b
        return b

    def dram(self, name, shape, dt):
        t = self.nc.dram_tensor(name, list(shape), dt, kind="Internal")
        return Buf(t.ap(), name)

    def _wait(self, e, ev):
        if ev is None:
            return
        k, v = ev
        if e == "pe" and k.startswith("s_pe_"):
            return
        if self.seen[e].get(k, 0) >= v:
            return
        self.prog[e].append(("w", self.sems[k], v))
        self.seen[e][k] = v

    def _deps(self, e, reads, writes):
        for b in reads:
            self._wait(e, b.lw)
        for b in writes:
            self._wait(e, b.lw)
            for k, v in list(b.rd.items()):
                self._wait(e, (k, v))

    def _mark(self, ev, reads, writes):
        k, v = ev
        for b in reads:
            if b.rd.get(k, 0) < v:
                b.rd[k] = v
        for b in writes:
            b.lw = ev
            b.rd = {}

    def op(self, e, fn, reads=(), writes=()):
        self._deps(e, reads, writes)
        self.ninstr[e] += 1
        c = self.cur[e]
        self.prog[e].append(("i", fn, self.sems[c[0]], 1))
        c[1] += 1
        self._mark((c[0], c[1]), reads, writes)
        if c[1] >= EPOCH:
            self._new_epoch(e)

    def dma_op(self, e, fn, reads=(), writes=()):
        pool, idx = self.dma[e]
        slot = pool[idx % NDMASEM]
        self.dma[e][1] += 1
        if slot[1] > 0:
            self._wait(e, (slot[0], slot[1]))
        self._deps(e, reads, writes)
        self.ninstr[e] += 1
        self.prog[e].append(("i", fn, self.sems[slot[0]], 16))
        slot[1] += 16
        self._mark((slot[0], slot[1]), reads, writes)

    def finish(self, bufs):
        for b in bufs:
            self._wait("sp", b.lw)


class Prog:
    def __init__(self, layers, final_norm, dbg_peer_only=False):
        self.layers = layers
        self.final_norm = final_norm
        self.dbg_peer_only = dbg_peer_only
        nc = bass.Bass("TRN2", target_bir_lowering=False)
        self.nc = nc
        self.kb = KB(nc)
        kb = self.kb
        di = {}

        def inp(name, shape, dt=F32):
            di[name] = Buf(nc.dram_tensor("i_" + name, list(shape), dt, kind="ExternalInput").ap(), name)
            return di[name]
        self.di = di
        inp("x", [S, D])
        inp("ccol", [128, 8])
        inp("ident", [128, 128])
        inp("ada_w", [DEPTH, D, 6 * D])
        inp("ada_b", [DEPTH, 1, 6 * D])
        inp("norm_mix", [DEPTH, 1, D])
        inp("norm_ffn", [DEPTH, 1, D])
        inp("norm_final", [1, D])
        inp("peer_w_q", [DEPTH, D, 2048])
        inp("peer_skT", [DEPTH, 2, 128, 128])
        inp("peer_uv", [DEPTH * 16384, 2 * D])
        inp("iota16", [128, 16])
        inp("odd_w_in", [2, D, 3088])
        inp("odd_b_forget", [2, 16, 1])
        inp("odd_w_out", [2, D, D])
        inp("sel127", [128, 128])
        inp("even_w_in", [2, D, 2888])
        inp("even_w_out", [2, D, D])
        inp("a_kv_norm", [2, 1, 256])
        inp("ukT", [2, 8, 64, 256])
        inp("a_w_uv", [2, 256, 512])
        inp("b_lambda", [2, 4, 64])
        inp("b_subln", [2, 128, 1])
        inp("b31rep", [128, 12])
        inp("biasT", [128, 24, 128])
        inp("negtri", [128, 128])
        inp("tri", [128, 128])
        self.out = Buf(nc.dram_tensor("out", [S, D], F32, kind="ExternalOutput").ap(), "out")
        self.MODBC = kb.dram("modbc", [DEPTH, 128, 6 * D], F32)
        self.XA = kb.dram("xa", [S, D], F32)
        self.XB = kb.dram("xb", [S, D], F32)
        self.UV16 = kb.dram("uv16", [DEPTH * 16384, 2 * D], BF16)
        self.UV16L = [Buf(self.UV16.t, f"uv16_{l}") for l in range(DEPTH)]
        self._uvdone = set()
        self.QC = kb.dram("qc", [8, 128, S], BF16)
        self.KC = kb.dram("kc", [8, 128, S], BF16)
        self.VC = kb.dram("vc", [S, D], BF16)
        self.OT = kb.dram("ot", [16, 64, S], BF16)
        self.CQ = kb.dram("cq", [16, S], BF16)
        self.QLAT = kb.dram("qlat", [16, 128, S], BF16)
        self.CKV = kb.dram("ckv", [S, 256], BF16)
        self.CKVT = kb.dram("ckvt", [2, 128, S], BF16)
        self.QI = kb.dram("qi", [8, 64, S], BF16)
        self.KI = kb.dram("ki", [64, S], BF16)
        self.WI = kb.dram("wi", [S, 8], F32)
        self.QB = kb.dram("qb", [4, 128, S], BF16)
        self.KBd = kb.dram("kbd", [4, 128, S], BF16)
        self.VB = kb.dram("vb", [S, 512], BF16)
        self.MT = kb.dram("mt", [32, 128, S], BF16)
        self.OTA = kb.dram("ota", [8, 64, S], BF16)
        self.OTB = kb.dram("otb", [4, 128, S], BF16)
        self.PS = [kb.ps(f"ps{i}", [128, 512], F32) for i in range(7)]
        self.PSB = kb.ps("psb", [128, 1024], BF16)
        self.ident = kb.sb("ident", [128, 128], F32, persist=True)
        self.identb = kb.sb("identb", [128, 128], BF16, persist=True)
        self.ones_row = kb.sb("ones_row", [1, 128], F32, persist=True)
        self.eps_col = kb.sb("eps_col", [128, 1], F32, persist=True)
        kb.dma_op("sp", lambda: nc.sync.dma_start(out=self.ident[:, :], in_=di["ident"][:, :]),
                  reads=[di["ident"]], writes=[self.ident])
        kb.op("dve", lambda: nc.vector.tensor_copy(out=self.identb[:, :], in_=self.ident[:, :]),
              reads=[self.ident], writes=[self.identb])
        kb.op("dve", lambda: nc.vector.memset(self.ones_row[:, :], 1.0), writes=[self.ones_row])
        kb.op("dve", lambda: nc.vector.memset(self.eps_col[:, :], EPS), writes=[self.eps_col])

    def barrier(self, reset=True):
        kb = self.kb
        evs = []
        for e in kb.eng:
            k, c = kb.cur[e]
            if c > 0:
                evs.append((k, c))
        for e in ("sp", "pool"):
            for slot in kb.dma[e][0]:
                if slot[1] > 0:
                    evs.append((slot[0], slot[1]))
        for e in kb.eng:
            for ev in evs:
                if ev[0].startswith(f"s_{e}_"):
                    continue
                kb._wait(e, ev)
        if reset:
            kb.phase_reset()

    def convert_uv(self, l):
        if l in self._uvdone:
            return
        self._uvdone.add(l)
        nc, kb = self.nc, self.kb
        k = f"cv_{l}"
        kb.sems[k] = nc.alloc_semaphore(name=k)
        src = self.di["peer_uv"]
        dst = self.UV16.t
        nd = 32
        rows = 16384 // nd
        for i in range(nd):
            r0 = l * 16384 + i * rows
            kb.prog["pool"].append(("i", (lambda r0=r0: nc.gpsimd.dma_start(out=dst[r0:r0 + rows, :], in_=src[r0:r0 + rows, :])),
                                    kb.sems[k], 16))
        self.UV16L[l].lw = (k, 16 * nd)

    def mm(self, out_b, out_ap, l_b, l_ap, r_b, r_ap, start, stop):
        nc = self.nc
        self.kb.op("pe", lambda: nc.tensor.matmul(out_ap, lhsT=l_ap, rhs=r_ap, start=start, stop=stop),
                   reads=[l_b, r_b], writes=[out_b])

    def load(self, dst_b, dst_ap, src_b, src_ap, q="sp"):
        nc = self.nc
        eng = nc.sync if q == "sp" else nc.gpsimd
        self.kb.dma_op(q, lambda: eng.dma_start(out=dst_ap, in_=src_ap), reads=[src_b], writes=[dst_b])

    def phase_mods(self):
        nc, kb, di = self.nc, self.kb, self.di
        ccol = kb.sb("ccol", [128, 8], F32)
        cact = kb.sb("cact", [128, 8], F32)
        cbc = Buf(kb.sb("xo", [128, D], F32)[:, :].rearrange("p (k t) -> p k t", k=8), "cbc")
        self.load(ccol, ccol[:, :], di["ccol"], di["ccol"][:, :])
        kb.op("act", lambda: nc.scalar.activation(out=cact[:, :], in_=ccol[:, :], func=AF.Silu),
              reads=[ccol], writes=[cact])
        kb.op("dve", lambda: nc.vector.tensor_copy(out=cbc[:, :, :], in_=cact[:, :].unsqueeze(2).to_broadcast([128, 8, 128])),
              reads=[cact], writes=[cbc])
        brow = kb.sb("brow", [1, 512], F32)
        gall = kb.sb("gbuf_all", [128, 6176], F32)
        wst = [Buf(gall[:, i * 3088:(i + 1) * 3088], f"wst{i}") for i in range(2)]
        mo = Buf(kb.sb("qT", [128, 16, 256], F32)[:, :, :].rearrange("p a b -> p (a b)"), "mo")
        n = 0
        for l in self.layers:
            for half in range(2):
                c0 = half * 3072
                for kc in range(8):
                    w = wst[n % 2]
                    n += 1
                    self.load(w, w[:, :3072], di["ada_w"], di["ada_w"][l, kc * 128:(kc + 1) * 128, c0:c0 + 3072])
                    for j in range(6):
                        self.mm(self.PS[j], self.PS[j][:, :], cbc, cbc[:, kc, :], w, w[:, j * 512:(j + 1) * 512],
                                start=(kc == 0), stop=False)
                for j in range(6):
                    self.load(brow, brow[:, :], di["ada_b"], di["ada_b"][l, :, c0 + j * 512:c0 + (j + 1) * 512])
                    self.mm(self.PS[j], self.PS[j][:, :], self.ones_row, self.ones_row[:, :],
                            brow, brow[:, :], start=False, stop=True)
                for j in range(6):
                    pj = self.PS[j]
                    if j % 2 == 0:
                        kb.op("act", lambda pj=pj, j=j: nc.scalar.copy(out=mo[:, j * 512:(j + 1) * 512], in_=pj[:, :]),
                              reads=[pj], writes=[mo])
                    else:
                        kb.op("dve", lambda pj=pj, j=j: nc.vector.tensor_copy(out=mo[:, j * 512:(j + 1) * 512], in_=pj[:, :]),
                              reads=[pj], writes=[mo])
                self.load(self.MODBC, self.MODBC[l, :, c0:c0 + 3072], mo, mo[:, 0:3072])

    def load_modvecs(self, l, which, norm_name):
        nc, kb, di = self.nc, self.kb, self.di
        gv = kb.sb("gvec", [128, D], F32)
        sv = kb.sb("shvec", [128, D], F32)
        gate = kb.sb("gatevec", [128, D], F32)
        nv = kb.sb("xo", [128, D], F32)
        base = which * 3 * D
        self.load(sv, sv[:, :], self.MODBC, self.MODBC[l, :, base:base + D])
        self.load(gv, gv[:, :], self.MODBC, self.MODBC[l, :, base + D:base + 2 * D])
        self.load(gate, gate[:, :], self.MODBC, self.MODBC[l, :, base + 2 * D:base + 3 * D])
        self.load(nv, nv[:, :], di[norm_name], di[norm_name][l, :, :].partition_broadcast(128))
        kb.op("dve", lambda: nc.vector.scalar_tensor_tensor(out=gv[:, :], in0=gv[:, :], scalar=1.0, in1=nv[:, :],
                                                            op0=ALU.add, op1=ALU.mult),
              reads=[gv, nv], writes=[gv])
        return gv, sv, gate

    def norm_tile(self, xt, gv, sv, htok, h16):
        nc, kb = self.nc, self.kb
        junk = kb.sb("junk", [128, D], BF16)
        ssq = kb.sb("ssq", [128, 1], F32)
        rstd = kb.sb("rstd", [128, 1], F32)
        kb.op("act", lambda: nc.scalar.activation(out=junk[:, :], in_=xt[:, :], func=AF.Square, accum_out=ssq[:, :]),
              reads=[xt], writes=[junk, ssq])
        kb.op("act", lambda: nc.scalar.activation(out=rstd[:, :], in_=ssq[:, :], func=AF.Sqrt, scale=1.0 / D,
                                                  bias=self.eps_col[:, :]),
              reads=[ssq, self.eps_col], writes=[rstd])
        kb.op("dve", lambda: nc.vector.reciprocal(out=rstd[:, :], in_=rstd[:, :]), reads=[rstd], writes=[rstd])
        kb.op("dve", lambda: nc.vector.scalar_tensor_tensor(out=htok[:, :], in0=xt[:, :], scalar=rstd[:, :], in1=gv[:, :],
                                                            op0=ALU.mult, op1=ALU.mult),
              reads=[xt, rstd, gv], writes=[htok])
        kb.op("dve", lambda: nc.vector.tensor_tensor(out=htok[:, :], in0=htok[:, :], in1=sv[:, :], op=ALU.add),
              reads=[htok, sv], writes=[htok])
        if h16 is not None:
            kb.op("act", lambda: nc.scalar.copy(out=h16[:, :], in_=htok[:, :]), reads=[htok], writes=[h16])

    def transpose_tile(self, h16, hT, col0):
        nc, kb = self.nc, self.kb
        for kc in range(8):
            kb.op("pe", lambda kc=kc: nc.tensor.transpose(self.PSB[:, kc * 128:(kc + 1) * 128],
                                                          h16[:, kc * 128:(kc + 1) * 128], self.identb[:, :]),
                  reads=[h16, self.identb], writes=[self.PSB])
        kb.op("dve", lambda: nc.vector.tensor_copy(out=hT[:, :, col0:col0 + 128],
                                                   in_=self.PSB[:, :].rearrange("p (k t) -> p k t", k=8)),
              reads=[self.PSB], writes=[hT])

    def load_w16(self, w16, src_b, src_ap_fn, ncols, stage=None):
        nc, kb = self.nc, self.kb
        if stage is None:
            gall = kb.sb("gbuf_all", [128, 6176], F32)
            st = [Buf(gall[:, i * 3088:(i + 1) * 3088], f"wst{i}") for i in range(2)]
        else:
            qT_ = kb.sb("qT", [128, 16, 256], F32)
            st = [Buf(qT_[:, i * 8:(i + 1) * 8, :].rearrange("p a b -> p (a b)"), f"wstg{i}") for i in range(2)]
        for kc in range(8):
            s = st[kc % 2]
            self.load(s, s[:, :ncols], src_b, src_ap_fn(kc))
            if kc % 2 == 0:
                kb.op("dve", lambda s=s, kc=kc: nc.vector.tensor_copy(out=w16[:, kc, :ncols], in_=s[:, :ncols]),
                      reads=[s], writes=[w16])
            else:
                kb.op("act", lambda s=s, kc=kc: nc.scalar.copy(out=w16[:, kc, :ncols], in_=s[:, :ncols]),
                      reads=[s], writes=[w16])


    def inproj_blocks(self, XIN, gv, sv, w16, fm_specs, tm_specs, hT, extra=None):
        nc, kb = self.nc, self.kb
        xts = [kb.sb(f"xt{i}", [128, D], F32) for i in range(2)]
        hts = [kb.sb(f"htok{i}", [128, D], F32) for i in range(2)]
        h16 = [kb.sb(f"h16_{i}", [128, D], BF16) for i in range(2)]
        n = 0
        for blk in range(S // 256):
            for tt in range(2):
                ti = blk * 2 + tt
                xt = xts[tt]
                self.load(xt, xt[:, :], XIN, XIN[ti * 128:(ti + 1) * 128, :])
                self.norm_tile(xt, gv, sv, hts[tt], h16[tt])
                self.transpose_tile(h16[tt], hT, tt * 128)
            for (col0, M, fn) in fm_specs:
                pb = self.PS[n % 3]
                n += 1
                for kc in range(8):
                    self.mm(pb, pb[0:M, 0:256], w16, w16[:, kc, col0:col0 + M], hT, hT[:, kc, :],
                            start=(kc == 0), stop=(kc == 7))
                fn(pb, blk)
            for tt in range(2):
                ti = blk * 2 + tt
                for (col0, ncols, fn) in tm_specs:
                    pb = self.PS[3 + n % 3]
                    n += 1
                    for kc in range(8):
                        self.mm(pb, pb[:, 0:ncols], hT, hT[:, kc, tt * 128:(tt + 1) * 128], w16, w16[:, kc, col0:col0 + ncols],
                                start=(kc == 0), stop=(kc == 7))
                    fn(pb, ti)
            if extra is not None:
                extra(blk)

    def outproj(self, l, XIN, XOUT, gate, w_out_b, w_out_ap, chunks, OT_loader):
        nc, kb = self.nc, self.kb
        nch = len(chunks)
        wo = kb.sb("w16", [128, 8, 3088], BF16)
        wov = Buf(wo[:, :, :].rearrange("p a b -> p (a b)"), "wov")
        gall = kb.sb("gbuf_all", [128, 6176], F32)
        st = [Buf(gall[:, i * 1024:(i + 1) * 1024], f"wos{i}") for i in range(4)]
        for ci, (K, r0) in enumerate(chunks):
            sbuf = st[ci % 4]
            self.load(sbuf, sbuf[0:K, :], w_out_b, w_out_ap(r0, K))
            if ci % 2 == 0:
                kb.op("dve", lambda sbuf=sbuf, ci=ci, K=K: nc.vector.tensor_copy(out=wov[0:K, ci * 1024:(ci + 1) * 1024], in_=sbuf[0:K, :]),
                      reads=[sbuf], writes=[wov])
            else:
                kb.op("act", lambda sbuf=sbuf, ci=ci, K=K: nc.scalar.copy(out=wov[0:K, ci * 1024:(ci + 1) * 1024], in_=sbuf[0:K, :]),
                      reads=[sbuf], writes=[wov])
        self.barrier(reset=False)
        xts = [kb.sb(f"xt{i}", [128, D], F32) for i in range(2)]
        xo = [kb.sb(f"htok{i}", [128, D], F32) for i in range(2)]
        for ti in range(NT):
            xt = xts[ti % 2]
            self.load(xt, xt[:, :], XIN, XIN[ti * 128:(ti + 1) * 128, :])
            obaps = OT_loader(ti)
            for half in range(2):
                pb = self.PS[(ti % 2) * 2 + half]
                for ci, (K, r0) in enumerate(chunks):
                    self.mm(pb, pb[:, :], obaps[ci][0], obaps[ci][1], wov, wov[0:K, ci * 1024 + half * 512: ci * 1024 + (half + 1) * 512],
                            start=(ci == 0), stop=(ci == nch - 1))
            o = xo[ti % 2]
            for half in range(2):
                pb = self.PS[(ti % 2) * 2 + half]
                kb.op("dve", lambda pb=pb, o=o, half=half: nc.vector.tensor_tensor(
                    out=o[:, half * 512:(half + 1) * 512], in0=pb[:, :], in1=gate[:, half * 512:(half + 1) * 512], op=ALU.mult),
                    reads=[pb, gate], writes=[o])
            kb.op("dve", lambda o=o, xt=xt: nc.vector.tensor_tensor(out=o[:, :], in0=o[:, :], in1=xt[:, :], op=ALU.add),
                  reads=[o, xt], writes=[o])
            self.load(XOUT, XOUT[ti * 128:(ti + 1) * 128, :], o, o[:, :])

    def attn_block(self, I, KT, kt_aps, Q, q_aps, V, v_aps, dvs, bias_ap, mask_fn, ones_b, extra=None, clamp_diag=False, unit_mask_fn=None):
        nc, kb = self.nc, self.kb
        pts = [kb.sb(f"pt{i}", [128, 512], BF16) for i in range(4)]
        rz = kb.sb("rz", [128, 512], F32)
        nv = len(dvs)
        osb = [[kb.sb(f"osb{c}_{i}", [128, 512], BF16) for c in range(nv)] for i in range(2)]
        n = self._attn_n
        par = self._attn_par
        self._attn_par ^= 1
        pOs = [self.PS[2 + c] for c in range(nv)]
        pZ = self.PS[4]
        jmax = 4 * I + 3

        def emit_sc(j):
            nonlocal n
            i0 = max(4 * I, j)
            c0 = (i0 - 4 * I) * 128
            psc = self.PS[(0, 1, 6)[n % 3]]
            pt = pts[n % 4]
            n += 1
            ks = kt_aps(j)
            qs = q_aps(I * 512 + c0, (I + 1) * 512)
            for kk in range(len(ks)):
                self.mm(psc, psc[:, c0:512], KT, ks[kk], Q, qs[kk], start=(kk == 0), stop=(kk == len(ks) - 1 and extra is None))
            if extra is not None:
                lb, lap, rb, rapf = extra
                self.mm(psc, psc[:, c0:512], lb, lap, rb, rapf(I * 512 + c0, (I + 1) * 512), start=False, stop=True)
            return (j, i0, c0, psc, pt)

        queue = [emit_sc(0)]
        if jmax >= 1:
            queue.append(emit_sc(1))
        for j in range(jmax + 1):
            (_, i0, c0, psc, pt) = queue.pop(0)
            b = bias_ap(I, j)
            breads = [psc] + ([b[0]] if isinstance(b, tuple) else [])
            bval = b[1] if isinstance(b, tuple) else b
            a0 = c0
            if clamp_diag and j >= 4 * I:
                tmpc = kb.sb("tmpc", [128, 128], F32)
                kb.op("dve", lambda psc=psc, c0=c0, bval=bval: nc.vector.tensor_scalar(
                    out=tmpc[:, :], in0=psc[:, c0:c0 + 128], scalar1=bval, scalar2=40.0, op0=ALU.add, op1=ALU.min),
                    reads=breads, writes=[tmpc])
                kb.op("act", lambda pt=pt, c0=c0: nc.scalar.activation(out=pt[:, c0:c0 + 128], in_=tmpc[:, :], func=AF.Exp),
                      reads=[tmpc], writes=[pt])
                a0 = c0 + 128
            if a0 < 512:
                kb.op("act", lambda psc=psc, pt=pt, a0=a0, bval=bval: nc.scalar.activation(
                    out=pt[:, a0:512], in_=psc[:, a0:512], func=AF.Exp, bias=bval),
                    reads=breads, writes=[pt])
            if unit_mask_fn is not None:
                unit_mask_fn(pt, c0, I, j)
            for i in range(i0, 4 * I + 4):
                mask_fn(pt, (i - 4 * I) * 128, i, j)
            if j + 2 <= jmax:
                queue.append(emit_sc(j + 2))
            vs_ = v_aps(j)
            for c in range(nv):
                self.mm(pOs[c], pOs[c][0:dvs[c], c0:512], V, vs_[c], pt, pt[:, c0:512], start=(j == 0), stop=(j == jmax))
            self.mm(pZ, pZ[:, c0:512], ones_b, ones_b[:, :], pt, pt[:, c0:512], start=(j == 0), stop=(j == jmax))
        kb.op("dve", lambda: nc.vector.reciprocal(out=rz[:, :], in_=pZ[:, :]), reads=[pZ], writes=[rz])
        outs = []
        for c in range(nv):
            ob = osb[par][c]
            kb.op("dve", lambda ob=ob, c=c: nc.vector.tensor_tensor(out=ob[0:dvs[c], :], in0=pOs[c][0:dvs[c], :], in1=rz[0:dvs[c], :], op=ALU.mult),
                  reads=[pOs[c], rz], writes=[ob])
            outs.append(ob)
        self._attn_n = n
        return outs


    def prep_ctiles(self):
        if getattr(self, "_ctiles", None) is not None:
            return
        nc, kb, di = self.nc, self.kb, self.di
        ct = kb.sb("ctiles", [128, 24, 128], BF16, persist=True)
        b31 = kb.sb("b31", [128, 12], F32, persist=True)
        nb31 = kb.sb("nb31", [128, 12], F32, persist=True)
        self.load(b31, b31[:, :], di["b31rep"], di["b31rep"][:, :])
        kb.op("dve", lambda: nc.vector.tensor_scalar(out=nb31[:, :], in0=b31[:, :], scalar1=-1.0, scalar2=None, op0=ALU.mult),
              reads=[b31], writes=[nb31])
        tri32 = kb.sb("tri32", [128, 128], F32)
        self.load(tri32, tri32[:, :], di["tri"], di["tri"][:, :])
        bt = kb.sb("btst", [128, 24, 128], F32)
        self.load(bt, bt[:, :, :], di["biasT"], di["biasT"][:, :, :])
        for h in range(12):
            for dl in range(2):
                k = h * 2 + dl
                kb.op("act", lambda k=k, h=h: nc.scalar.activation(out=bt[:, k, :], in_=bt[:, k, :], func=AF.Exp, bias=nb31[:, h:h + 1]),
                      reads=[bt, nb31], writes=[bt])
                if dl == 0:
                    kb.op("dve", lambda k=k: nc.vector.tensor_tensor(out=ct[:, k, :], in0=bt[:, k, :], in1=tri32[:, :], op=ALU.mult),
                          reads=[bt, tri32], writes=[ct])
                else:
                    kb.op("dve", lambda k=k: nc.vector.tensor_copy(out=ct[:, k, :], in_=bt[:, k, :]), reads=[bt], writes=[ct])
        self._ctiles = (ct, b31)
        self.barrier()

    def phase_even(self, l, XIN, XOUT):
        nc, kb, di = self.nc, self.kb, self.di
        e = l // 2
        lam_init = 0.8 - 0.6 * math.exp(-0.3 * l)
        self.prep_ctiles()
        ct, b31 = self._ctiles
        gv, sv, gate = self.load_modvecs(l, 0, "norm_mix")
        w16 = kb.sb("w16", [128, 8, 3088], BF16)
        self.load_w16(w16, di["even_w_in"], lambda kc: di["even_w_in"][e, kc * 128:(kc + 1) * 128, :], 2888)
        gall = kb.sb("gbuf_all", [128, 6176], F32)
        ukst = Buf(gall[0:64, 0:2048].rearrange("p (h c) -> p h c", h=8), "ukst")
        ukT = kb.sb("ukT", [64, 8, 256], BF16)
        self.barrier(reset=False)
        self.load(ukst, ukst[:, :, :], di["ukT"], di["ukT"][e, :, :, :].rearrange("h d c -> d h c"))
        kb.op("dve", lambda: nc.vector.tensor_copy(out=ukT[:, :, :], in_=ukst[:, :, :]), reads=[ukst], writes=[ukT])
        kvn = kb.sb("kvn", [128, 256], F32)
        self.load(kvn, kvn[:, :], di["a_kv_norm"], di["a_kv_norm"][e, :, :].partition_broadcast(128))
        self.barrier(reset=False)
        hT = kb.sb("hT", [128, 8, 256], BF16)
        QLAT, CKV, CKVT, QI, KI, WI, QB, KBd, VB = (self.QLAT, self.CKV, self.CKVT, self.QI, self.KI, self.WI,
                                                     self.QB, self.KBd, self.VB)
        qah = [kb.sb(f"qah{i}", [64, 256], BF16) for i in range(2)]
        qlst = [kb.sb(f"qlst{i}", [128, 16, 256], BF16) for i in range(2)]
        qist = [kb.sb(f"qist{i}", [64, 8, 256], BF16) for i in range(2)]
        kist = [kb.sb(f"kist{i}", [64, 256], BF16) for i in range(2)]
        qbst = [kb.sb(f"qbst{i}", [128, 4, 256], BF16) for i in range(2)]
        kbst = [kb.sb(f"kbst{i}", [128, 4, 256], BF16) for i in range(2)]
        vbst = [kb.sb(f"vbst{i}", [128, 512], BF16) for i in range(2)]
        ckst = [kb.sb(f"ckst{i}", [128, 256], BF16) for i in range(2)]
        cktst = [kb.sb(f"cktst{i}", [128, 2, 128], BF16) for i in range(2)]
        wist = [kb.sb(f"wist{i}", [128, 8], F32) for i in range(2)]
        fm, tm = [], []
        for h in range(8):
            def fqa(pb, blk, h=h):
                qa = qah[h % 2]
                ql = qlst[blk % 2]
                kb.op("act", lambda: nc.scalar.copy(out=qa[:, :], in_=pb[0:64, 0:256]), reads=[pb], writes=[qa])
                for cc in range(2):
                    p2 = self.PS[6]
                    self.mm(p2, p2[:, 0:256], ukT, ukT[:, h, cc * 128:(cc + 1) * 128], qa, qa[:, :], start=True, stop=True)
                    kb.op("dve", lambda cc=cc, p2=p2: nc.vector.tensor_scalar(out=ql[:, h * 2 + cc, :], in0=p2[:, 0:256], scalar1=0.125,
                                                                              scalar2=None, op0=ALU.mult), reads=[p2], writes=[ql])
                if h == 7:
                    self.load(QLAT, QLAT[:, :, blk * 256:(blk + 1) * 256].rearrange("m p t -> p m t"), ql, ql[:, :, :])
            fm.append((h * 64, 64, fqa))
        for h in range(8):
            def fqi(pb, blk, h=h):
                q = qist[blk % 2]
                kb.op("act", lambda: nc.scalar.copy(out=q[:, h, :], in_=pb[0:64, 0:256]), reads=[pb], writes=[q])
                if h == 7:
                    self.load(QI, QI[:, :, blk * 256:(blk + 1) * 256].rearrange("h p t -> p h t"), q, q[:, :, :])
            fm.append((768 + h * 64, 64, fqi))

        def fki(pb, blk):
            k = kist[blk % 2]
            kb.op("dve", lambda: nc.vector.tensor_copy(out=k[:, :], in_=pb[0:64, 0:256]), reads=[pb], writes=[k])
            self.load(KI, KI[:, blk * 256:(blk + 1) * 256], k, k[:, :])
        fm.append((1280, 64, fki))
        for h in range(4):
            def fqb(pb, blk, h=h):
                q = qbst[blk % 2]
                kb.op("act", lambda: nc.scalar.activation(out=q[:, h, :], in_=pb[:, 0:256], func=AF.Copy, scale=0.125),
                      reads=[pb], writes=[q])
                if h == 3:
                    self.load(QB, QB[:, :, blk * 256:(blk + 1) * 256].rearrange("h p t -> p h t"), q, q[:, :, :])
            fm.append((1352 + h * 128, 128, fqb))
        for h in range(4):
            def fkb(pb, blk, h=h):
                k = kbst[blk % 2]
                kb.op("dve", lambda: nc.vector.tensor_copy(out=k[:, h, :], in_=pb[:, 0:256]), reads=[pb], writes=[k])
                if h == 3:
                    self.load(KBd, KBd[:, :, blk * 256:(blk + 1) * 256].rearrange("h p t -> p h t"), k, k[:, :, :])
            fm.append((1864 + h * 128, 128, fkb))
        junkc = kb.sb("junkc", [128, 256], BF16)
        ssqc = kb.sb("ssqc", [128, 1], F32)
        ckf = kb.sb("ckf", [128, 256], F32)

        def fckv(pb, ti):
            ck = ckst[ti % 2]
            ckt = cktst[ti % 2]
            kb.op("act", lambda: nc.scalar.activation(out=junkc[:, :], in_=pb[:, 0:256], func=AF.Square, accum_out=ssqc[:, :]),
                  reads=[pb], writes=[junkc, ssqc])
            kb.op("act", lambda: nc.scalar.activation(out=ssqc[:, :], in_=ssqc[:, :], func=AF.Sqrt, scale=1.0 / 256, bias=self.eps_col[:, :]),
                  reads=[ssqc, self.eps_col], writes=[ssqc])
            kb.op("dve", lambda: nc.vector.reciprocal(out=ssqc[:, :], in_=ssqc[:, :]), reads=[ssqc], writes=[ssqc])
            kb.op("dve", lambda: nc.vector.scalar_tensor_tensor(out=ck[:, :], in0=pb[:, 0:256], scalar=ssqc[:, :], in1=kvn[:, :],
                                                                op0=ALU.mult, op1=ALU.mult), reads=[pb, ssqc, kvn], writes=[ck])
            self.load(CKV, CKV[ti * 128:(ti + 1) * 128, :], ck, ck[:, :])
            for cc in range(2):
                kb.op("pe", lambda cc=cc: nc.tensor.transpose(self.PSB[:, cc * 128:(cc + 1) * 128], ck[:, cc * 128:(cc + 1) * 128], self.identb[:, :]),
                      reads=[ck, self.identb], writes=[self.PSB])
            kb.op("act", lambda: nc.scalar.copy(out=ckt[:, :, :], in_=self.PSB[:, 0:256].rearrange("p (c t) -> p c t", c=2)),
                  reads=[self.PSB], writes=[ckt])
            self.load(CKVT, CKVT[:, :, ti * 128:(ti + 1) * 128].rearrange("c p t -> p c t"), ckt, ckt[:, :, :])
        tm.append((512, 256, fckv))

        def fwi(pb, ti):
            w = wist[ti % 2]
            kb.op("dve", lambda: nc.vector.tensor_scalar(out=w[:, :], in0=pb[:, 0:8], scalar1=(8.0 ** -0.5) * 0.125, scalar2=None,
                                                         op0=ALU.mult), reads=[pb], writes=[w])
            self.load(WI, WI[ti * 128:(ti + 1) * 128, :], w, w[:, :])
        tm.append((1344, 8, fwi))

        def fvb(pb, ti):
            v = vbst[ti % 2]
            kb.op("act", lambda: nc.scalar.copy(out=v[:, :], in_=pb[:, :]), reads=[pb], writes=[v])
            self.load(VB, VB[ti * 128:(ti + 1) * 128, :], v, v[:, :])
        tm.append((2376, 512, fvb))
        self.inproj_blocks(XIN, gv, sv, w16, fm, tm, hT)
        self.barrier()
        MT = self.MT
        kiT = kb.sb("kiT", [64, S], BF16)
        self.load(kiT, kiT[:, :], KI, KI[:, :])
        negtri = kb.sb("negtri", [128, 128], F32)
        self.load(negtri, negtri[:, :], di["negtri"], di["negtri"][:, :])
        isc = [kb.sb(f"isc{i}", [128, S], F32) for i in range(2)]
        tmpw = [kb.sb(f"tmpw{i}", [128, 512], F32) for i in range(3)]
        qit = [kb.sb(f"qit{i}", [64, 8, 128], BF16) for i in range(2)]
        wit = [kb.sb(f"wit{i}", [128, 8], F32) for i in range(2)]
        mk = [kb.sb(f"mk{i}", [128, S], BF16) for i in range(2)]
        mts = [kb.sb(f"mts{i}", [128, 8, 128], BF16) for i in range(2)]
        jk = kb.sb("jk", [128, S], BF16)
        sc_ = {n_: kb.sb(f"bs_{n_}", [128, 1], F32) for n_ in ("lo", "hi", "mid", "cnt", "ge", "d1", "d2")}
        n = 0
        nm = 0
        for i in range(NT):
            L = (i + 1) * 128
            q = qit[i % 2]
            w = wit[i % 2]
            sc = isc[i % 2]
            self.load(q, q[:, :, :], QI, QI[:, :, i * 128:(i + 1) * 128].rearrange("h p t -> p h t"))
            self.load(w, w[:, :], WI, WI[i * 128:(i + 1) * 128, :])
            for kbk in range((L + 511) // 512):
                a = kbk * 512
                b_ = min(L, a + 512)
                wd = b_ - a
                for h in range(8):
                    pb = self.PS[n % 4]
                    n += 1
                    self.mm(pb, pb[:, 0:wd], q, q[:, h, :], kiT, kiT[:, a:b_], start=True, stop=True)
                    if h == 0:
                        kb.op("dve", lambda pb=pb, a=a, b_=b_, wd=wd, sc=sc, w=w: nc.vector.tensor_scalar(
                            out=sc[:, a:b_], in0=pb[:, 0:wd], scalar1=0.0, scalar2=w[:, 0:1], op0=ALU.max, op1=ALU.mult),
                            reads=[pb, w], writes=[sc])
                    else:
                        t_ = tmpw[n % 3]
                        kb.op("dve", lambda pb=pb, wd=wd, t_=t_, w=w, h=h: nc.vector.tensor_scalar(
                            out=t_[:, 0:wd], in0=pb[:, 0:wd], scalar1=0.0, scalar2=w[:, h:h + 1], op0=ALU.max, op1=ALU.mult),
                            reads=[pb, w], writes=[t_])
                        kb.op("pool", lambda a=a, b_=b_, wd=wd, t_=t_, sc=sc: nc.gpsimd.tensor_tensor(
                            out=sc[:, a:b_], in0=sc[:, a:b_], in1=t_[:, 0:wd], op=ALU.add), reads=[sc, t_], writes=[sc])
            lo, hi, mid, cnt, ge, d1, d2 = (sc_[k_] for k_ in ("lo", "hi", "mid", "cnt", "ge", "d1", "d2"))
            kb.op("dve", lambda sc=sc, L=L: nc.vector.tensor_reduce(out=lo[:, :], in_=sc[:, 0:L], axis=AX.X, op=ALU.min), reads=[sc], writes=[lo])
            kb.op("dve", lambda: nc.vector.tensor_scalar(out=lo[:, :], in0=lo[:, :], scalar1=-1.0, scalar2=None, op0=ALU.add), reads=[lo], writes=[lo])
            kb.op("dve", lambda sc=sc, L=L: nc.vector.tensor_tensor(out=sc[:, L - 128:L], in0=sc[:, L - 128:L], in1=negtri[:, :], op=ALU.add),
                  reads=[sc, negtri], writes=[sc])
            kb.op("dve", lambda sc=sc, L=L: nc.vector.tensor_reduce(out=hi[:, :], in_=sc[:, 0:L], axis=AX.X, op=ALU.max), reads=[sc], writes=[hi])
            kb.op("dve", lambda: nc.vector.tensor_scalar(out=hi[:, :], in0=hi[:, :], scalar1=1e-3, scalar2=None, op0=ALU.add), reads=[hi], writes=[hi])
            for it in range(16 if i >= 2 else 0):
                kb.op("dve", lambda: nc.vector.tensor_tensor(out=mid[:, :], in0=lo[:, :], in1=hi[:, :], op=ALU.add), reads=[lo, hi], writes=[mid])
                kb.op("dve", lambda: nc.vector.tensor_scalar(out=mid[:, :], in0=mid[:, :], scalar1=0.5, scalar2=None, op0=ALU.mult), reads=[mid], writes=[mid])
                kb.op("dve", lambda sc=sc, L=L: nc.vector.tensor_scalar(out=jk[:, 0:L], in0=sc[:, 0:L], scalar1=mid[:, 0:1], scalar2=0.0,
                                                                        op0=ALU.is_ge, op1=ALU.add, accum_out=cnt[:, :]),
                      reads=[sc, mid], writes=[jk, cnt])
                kb.op("dve", lambda: nc.vector.tensor_scalar(out=ge[:, :], in0=cnt[:, :], scalar1=255.5, scalar2=None, op0=ALU.is_ge), reads=[cnt], writes=[ge])
                kb.op("dve", lambda: nc.vector.tensor_tensor(out=d1[:, :], in0=mid[:, :], in1=lo[:, :], op=ALU.subtract), reads=[mid, lo], writes=[d1])
                kb.op("dve", lambda: nc.vector.tensor_tensor(out=d2[:, :], in0=hi[:, :], in1=mid[:, :], op=ALU.subtract), reads=[hi, mid], writes=[d2])
                kb.op("dve", lambda: nc.vector.scalar_tensor_tensor(out=lo[:, :], in0=d1[:, :], scalar=ge[:, 0:1], in1=lo[:, :], op0=ALU.mult, op1=ALU.add),
                      reads=[d1, ge, lo], writes=[lo])
                kb.op("dve", lambda: nc.vector.scalar_tensor_tensor(out=hi[:, :], in0=d2[:, :], scalar=ge[:, 0:1], in1=mid[:, :], op0=ALU.mult, op1=ALU.add),
                      reads=[d2, ge, mid], writes=[hi])
            m_ = mk[i % 2]
            kb.op("dve", lambda sc=sc, L=L, m_=m_: nc.vector.tensor_scalar(out=m_[:, 0:L], in0=sc[:, 0:L], scalar1=lo[:, 0:1], scalar2=None, op0=ALU.is_ge),
                  reads=[sc, lo], writes=[m_])
            for j0 in range(0, i + 1, 8):
                j1 = min(i + 1, j0 + 8)
                mt = mts[nm % 2]
                nm += 1
                for j in range(j0, j1):
                    kb.op("pe", lambda j=j, j0=j0, m_=m_: nc.tensor.transpose(self.PSB[:, (j - j0) * 128:(j - j0 + 1) * 128], m_[:, j * 128:(j + 1) * 128], self.identb[:, :]),
                          reads=[m_, self.identb], writes=[self.PSB])
                nj = j1 - j0
                kb.op("act", lambda mt=mt, nj=nj: nc.scalar.copy(out=mt[:, 0:nj, :], in_=self.PSB[:, 0:nj * 128].rearrange("p (j t) -> p j t", j=nj)),
                      reads=[self.PSB], writes=[mt])
                self.load(MT, MT[j0:j1, :, i * 128:(i + 1) * 128].rearrange("j p t -> p j t"), mt, mt[:, 0:nj, :])
        self.barrier()
        onesk = kb.sb("onesk", [128, 128], BF16)
        kb.op("dve", lambda: nc.vector.memset(onesk[:, :], 1.0), writes=[onesk])
        ckT = kb.sb("ckT", [128, 2, S], BF16)
        self.load(ckT, ckT[:, :, :], CKVT, CKVT[:, :, :].rearrange("c p t -> p c t"))
        ckv = kb.sb("ckvV", [128, 32, 256], BF16)
        for jj in range(4):
            self.load(ckv, ckv[:, jj * 8:(jj + 1) * 8, :], CKV, CKV[jj * 1024:(jj + 1) * 1024, :].rearrange("(j p) d -> p j d", p=128))
        uvst = kb.sb("uvst", [128, 2, 512], F32)
        uv = kb.sb("uv", [128, 2, 512], BF16)
        self.load(uvst, uvst[:, :, :], di["a_w_uv"], di["a_w_uv"][e, :, :].rearrange("(c p) n -> p c n", p=128))
        kb.op("dve", lambda: nc.vector.tensor_copy(out=uv[:, :, :], in_=uvst[:, :, :]), reads=[uvst], writes=[uv])
        Ql = [kb.sb(f"Ql{i}", [128, 2, S], BF16) for i in range(2)]
        mtl = [kb.sb(f"mtl{i}", [128, 512], BF16) for i in range(3)]
        oas = [kb.sb(f"oas{i}", [64, 512], BF16) for i in range(2)]
        OTA, OTB = self.OTA, self.OTB
        self._attn_n = 0
        self._attn_par = 0
        nmt = 0
        for h in range(8):
            q = Ql[h % 2]
            self.load(q, q[:, :, :], QLAT, QLAT[h * 2:h * 2 + 2, :, :].rearrange("c p t -> p c t"))
            for I in range(8):

                def unit_mask(pt, c0, I_, j):
                    nonlocal nmt
                    mt = mtl[nmt % 3]
                    nmt += 1
                    self.load(mt, mt[:, c0:512], MT, MT[j, :, I_ * 512 + c0:(I_ + 1) * 512])
                    kb.op("pool", lambda: nc.gpsimd.tensor_tensor(out=pt[:, c0:512], in0=pt[:, c0:512], in1=mt[:, c0:512], op=ALU.mult),
                          reads=[pt, mt], writes=[pt])

                def mask_fn(pt, cs, i, j, h=h):
                    if i - j in (0, 1):
                        k = h * 2 + (i - j)
                        kb.op("dve", lambda: nc.vector.tensor_tensor(out=pt[:, cs:cs + 128], in0=pt[:, cs:cs + 128], in1=ct[:, k, :], op=ALU.mult),
                              reads=[pt, ct], writes=[pt])
                outs = self.attn_block(I, ckT, lambda j: [ckT[:, 0, j * 128:(j + 1) * 128], ckT[:, 1, j * 128:(j + 1) * 128]],
                                       q, lambda a, b, q=q: [q[:, 0, a:b], q[:, 1, a:b]],
                                       ckv, lambda j: [ckv[:, j, 0:128], ckv[:, j, 128:256]], [128, 128],
                                       lambda i, j, h=h: (b31, b31[:, h:h + 1]), mask_fn, onesk, unit_mask_fn=unit_mask)
                p5 = self.PS[5]
                for cc in range(2):
                    self.mm(p5, p5[0:64, :], uv, uv[:, cc, h * 64:(h + 1) * 64], outs[cc], outs[cc][:, :], start=(cc == 0), stop=(cc == 1))
                oa = oas[I % 2]
                kb.op("act", lambda oa=oa, p5=p5: nc.scalar.copy(out=oa[:, :], in_=p5[0:64, :]), reads=[p5], writes=[oa])
                self.load(OTA, OTA[h, :, I * 512:(I + 1) * 512], oa, oa[:, :])
        self.barrier()
        onesk = kb.sb("onesk", [128, 128], BF16)
        kb.op("dve", lambda: nc.vector.memset(onesk[:, :], 1.0), writes=[onesk])
        ones32 = kb.sb("ones32", [128, 128], F32)
        kb.op("dve", lambda: nc.vector.memset(ones32[:, :], 1.0), writes=[ones32])
        lamt = kb.sb("lamt", [128, 4, 64], F32)
        self.load(lamt, lamt[:, :, :].rearrange("p a b -> p (a b)"), di["b_lambda"], di["b_lambda"][e:e + 1, :, :].rearrange("o a b -> o (a b)").partition_broadcast(128))
        lp = kb.sb("lp", [128, 2, 64], F32)
        ls = kb.sb("ls", [128, 2], F32)
        neglam = kb.sb("neglam", [128, 1], F32)
        kb.op("dve", lambda: nc.vector.tensor_tensor(out=lp[:, :, :], in0=lamt[:, 0::2, :], in1=lamt[:, 1::2, :], op=ALU.mult), reads=[lamt], writes=[lp])
        kb.op("dve", lambda: nc.vector.tensor_reduce(out=ls[:, :], in_=lp[:, :, :], axis=AX.X, op=ALU.add), reads=[lp], writes=[ls])
        kb.op("act", lambda: nc.scalar.activation(out=ls[:, :], in_=ls[:, :], func=AF.Exp), reads=[ls], writes=[ls])
        kb.op("dve", lambda: nc.vector.tensor_tensor(out=neglam[:, :], in0=ls[:, 1:2], in1=ls[:, 0:1], op=ALU.subtract), reads=[ls], writes=[neglam])
        kb.op("dve", lambda: nc.vector.tensor_scalar(out=neglam[:, :], in0=neglam[:, :], scalar1=-lam_init, scalar2=None, op0=ALU.add), reads=[neglam], writes=[neglam])
        sg = kb.sb("sg", [128, 1], F32)
        self.load(sg, sg[:, :], di["b_subln"], di["b_subln"][e, :, :])
        kb.op("dve", lambda: nc.vector.tensor_scalar(out=sg[:, :], in0=sg[:, :], scalar1=(1.0 - lam_init), scalar2=None, op0=ALU.mult), reads=[sg], writes=[sg])
        KTb = [kb.sb(f"KTb{i}", [128, S], BF16) for i in range(2)]
        Qb = [[kb.sb(f"Qb{i}_{mp}", [128, S], BF16) for mp in range(2)] for i in range(2)]
        Vb = [kb.sb(f"Vb{i}", [128, 32, 128], BF16) for i in range(2)]
        for i in range(2):
            for mp in range(2):
                qq = Qb[i][mp]
                kb.op("pool", lambda qq=qq: nc.gpsimd.memset(qq[:, :], 0.0), writes=[qq])
        o1 = kb.sb("o1", [128, 512], F32)
        od = kb.sb("od", [128, 512], F32)
        osq = kb.sb("osq", [128, 512], F32)
        rs = kb.sb("rs", [128, 512], F32)
        obf = [kb.sb(f"obf{i}", [128, 512], BF16) for i in range(2)]
        for h in range(4):
            kt = KTb[h % 2]
            vp = Vb[h % 2]
            self.load(kt, kt[:, :], KBd, KBd[h, :, :])
            for mp in range(2):
                qq = Qb[h % 2][mp]
                self.load(qq, qq[mp * 64:(mp + 1) * 64, :], QB, QB[h, mp * 64:(mp + 1) * 64, :])
            for jj in range(4):
                self.load(vp, vp[:, jj * 8:(jj + 1) * 8, :], VB, VB[jj * 1024:(jj + 1) * 1024, h * 128:(h + 1) * 128].rearrange("(j p) d -> p j d", p=128))
            hb = 8 + h

            def mask_fn2(pt, cs, i, j, hb=hb):
                if i - j in (0, 1):
                    k = hb * 2 + (i - j)
                    kb.op("dve", lambda: nc.vector.tensor_tensor(out=pt[:, cs:cs + 128], in0=pt[:, cs:cs + 128], in1=ct[:, k, :], op=ALU.mult),
                          reads=[pt, ct], writes=[pt])
            for I in range(8):
                for mp in range(2):
                    qq = Qb[h % 2][mp]
                    outs = self.attn_block(I, kt, lambda j, kt=kt: [kt[:, j * 128:(j + 1) * 128]],
                                           qq, lambda a, b, qq=qq: [qq[:, a:b]],
                                           vp, lambda j, vp=vp: [vp[:, j, :]], [128],
                                           lambda i, j, hb=hb: (b31, b31[:, hb:hb + 1]), mask_fn2, onesk)
                    ob = outs[0]
                    if mp == 0:
                        kb.op("act", lambda ob=ob: nc.scalar.copy(out=o1[:, :], in_=ob[:, :]), reads=[ob], writes=[o1])
                    else:
                        kb.op("dve", lambda ob=ob: nc.vector.scalar_tensor_tensor(out=od[:, :], in0=ob[:, :], scalar=neglam[:, 0:1], in1=o1[:, :],
                                                                                  op0=ALU.mult, op1=ALU.add), reads=[ob, neglam, o1], writes=[od])
                kb.op("act", lambda: nc.scalar.activation(out=osq[:, :], in_=od[:, :], func=AF.Square), reads=[od], writes=[osq])
                p5 = self.PS[5]
                self.mm(p5, p5[:, :], ones32, ones32[:, :], osq, osq[:, :], start=True, stop=True)
                kb.op("act", lambda p5=p5: nc.scalar.activation(out=rs[:, :], in_=p5[:, :], func=AF.Sqrt, scale=1.0 / 128, bias=self.eps_col[:, :]),
                      reads=[p5, self.eps_col], writes=[rs])
                kb.op("dve", lambda: nc.vector.reciprocal(out=rs[:, :], in_=rs[:, :]), reads=[rs], writes=[rs])
                of = obf[I % 2]
                kb.op("dve", lambda of=of: nc.vector.scalar_tensor_tensor(out=of[:, :], in0=od[:, :], scalar=sg[:, 0:1], in1=rs[:, :],
                                                                          op0=ALU.mult, op1=ALU.mult), reads=[od, sg, rs], writes=[of])
                self.load(OTB, OTB[h, :, I * 512:(I + 1) * 512], of, of[:, :])
        self.barrier()
        gv, sv, gate = self.load_modvecs(l, 0, "norm_mix")
        otsa = [kb.sb(f"otsa{i}", [64, 8, 128], BF16) for i in range(2)]
        otsb = [kb.sb(f"otsb{i}", [128, 4, 128], BF16) for i in range(2)]

        def ot_loader(ti):
            oa = otsa[ti % 2]
            ob = otsb[ti % 2]
            self.load(oa, oa[:, :, :], OTA, OTA[:, :, ti * 128:(ti + 1) * 128].rearrange("h p t -> p h t"))
            self.load(ob, ob[:, :, :], OTB, OTB[:, :, ti * 128:(ti + 1) * 128].rearrange("h p t -> p h t"))
            return [(oa, oa[:, h, :]) for h in range(8)] + [(ob, ob[:, h, :]) for h in range(4)]
        self.outproj(l, XIN, XOUT, gate, di["even_w_out"], lambda r0, K: di["even_w_out"][e, r0:r0 + K, :],
                     [(64, h * 64) for h in range(8)] + [(128, 512 + h * 128) for h in range(4)], ot_loader)

    def phase_mixer(self, l, XIN, XOUT):
        self.convert_uv(l)
        if l % 2 == 0:
            return self.phase_even(l, XIN, XOUT)
        nc, kb, di = self.nc, self.kb, self.di
        o = l // 2
        gv, sv, gate = self.load_modvecs(l, 0, "norm_mix")
        w16 = kb.sb("w16", [128, 8, 3088], BF16)
        self.load_w16(w16, di["odd_w_in"], lambda kc: di["odd_w_in"][o, kc * 128:(kc + 1) * 128, :], 3088)
        self.barrier(reset=False)
        hT = kb.sb("hT", [128, 8, 256], BF16)
        QC, KC, VC, OT = self.QC, self.KC, self.VC, self.OT
        LF = kb.sb("LF", [16, S], F32)
        bfc = kb.sb("bfc", [16, 1], F32)
        self.load(bfc, bfc[:, :], di["odd_b_forget"], di["odd_b_forget"][o, :, :])
        qst = [kb.sb(f"qst{i}", [128, 8, 256], BF16) for i in range(2)]
        kst = [kb.sb(f"kst{i}", [128, 8, 256], BF16) for i in range(2)]
        vst = [kb.sb(f"vst{i}", [128, 1024], BF16) for i in range(2)]
        ft = kb.sb("ft", [16, 256], F32)
        fm, tm = [], []
        for m in range(8):
            def fq(pb, blk, m=m):
                q = qst[blk % 2]
                kb.op("act", lambda: nc.scalar.activation(out=q[:, m, :], in_=pb[:, 0:256], func=AF.Copy, scale=0.125),
                      reads=[pb], writes=[q])
                if m == 7:
                    self.load(QC, QC[:, :, blk * 256:(blk + 1) * 256].rearrange("m p t -> p m t"), q, q[:, :, :])
            fm.append((m * 128, 128, fq))
        for m in range(8):
            def fk(pb, blk, m=m):
                k = kst[blk % 2]
                kb.op("dve", lambda: nc.vector.tensor_copy(out=k[:, m, :], in_=pb[:, 0:256]), reads=[pb], writes=[k])
                if m == 7:
                    self.load(KC, KC[:, :, blk * 256:(blk + 1) * 256].rearrange("m p t -> p m t"), k, k[:, :, :])
            fm.append((1024 + m * 128, 128, fk))

        def ff(pb, blk):
            kb.op("dve", lambda: nc.vector.tensor_scalar(out=ft[:, :], in0=pb[0:16, 0:256], scalar1=bfc[:, :], scalar2=-1.0,
                                                         op0=ALU.add, op1=ALU.mult), reads=[pb, bfc], writes=[ft])
            kb.op("act", lambda: nc.scalar.activation(out=ft[:, :], in_=ft[:, :], func=AF.Exp), reads=[ft], writes=[ft])
            kb.op("act", lambda: nc.scalar.activation(out=ft[:, :], in_=ft[:, :], func=AF.Ln, bias=1.0), reads=[ft], writes=[ft])
            kb.op("dve", lambda: nc.vector.tensor_scalar(out=LF[:, blk * 256:(blk + 1) * 256], in0=ft[:, :], scalar1=-1.0,
                                                         scalar2=None, op0=ALU.mult), reads=[ft], writes=[LF])
        fm.append((3072, 16, ff))
        for half in range(2):
            def fv(pb, ti, half=half):
                v = vst[ti % 2]
                if half == 0:
                    kb.op("act", lambda: nc.scalar.copy(out=v[:, 0:512], in_=pb[:, :]), reads=[pb], writes=[v])
                else:
                    kb.op("dve", lambda: nc.vector.tensor_copy(out=v[:, 512:1024], in_=pb[:, :]), reads=[pb], writes=[v])
                    self.load(VC, VC[ti * 128:(ti + 1) * 128, :], v, v[:, :])
            tm.append((2048 + half * 512, 512, fv))
        self.inproj_blocks(XIN, gv, sv, w16, fm, tm, hT)
        onesb = kb.sb("ones16", [16, S], BF16)
        cum = kb.sb("cum", [16, S], F32)
        kb.op("dve", lambda: nc.vector.memset(onesb[:, :], 1.0), writes=[onesb])
        kb.op("dve", lambda: nc.vector.tensor_tensor_scan(out=cum[:, :], data0=onesb[:, :], data1=LF[:, :], initial=0.0,
                                                          op0=ALU.mult, op1=ALU.add), reads=[onesb, LF], writes=[cum])
        cqr = kb.sb("cqr", [16, 8, 512], BF16)
        kb.op("dve", lambda: nc.vector.tensor_tensor(
            out=cqr[:, :, :], in0=cum[:, :].rearrange("p (j t) -> p j t", t=512),
            in1=cum[:, :].rearrange("p (j t) -> p j t", t=512)[:, :, 511:512].to_broadcast([16, 8, 512]), op=ALU.subtract),
            reads=[cum], writes=[cqr])
        self.load(self.CQ, self.CQ[:, :], cqr, cqr[:, :, :].rearrange("p j t -> p (j t)"))
        cumT = kb.sb("cumT", [128, 32, 16], F32, persist=True)
        refbc = kb.sb("refbc", [128, 32, 16], F32, persist=True)
        pc = self.PS[0]
        for j in range(32):
            kb.op("pe", lambda j=j: nc.tensor.transpose(pc[:, j * 16:(j + 1) * 16], cum[:, j * 128:(j + 1) * 128], self.ident[0:16, 0:16]),
                  reads=[cum, self.ident], writes=[pc])
        kb.op("dve", lambda: nc.vector.tensor_copy(out=cumT[:, :, :], in_=pc[:, :].rearrange("p (j h) -> p j h", h=16)),
              reads=[pc], writes=[cumT])
        sel = kb.sb("sel127", [128, 128], F32)
        self.load(sel, sel[:, :], di["sel127"], di["sel127"][:, :])
        pr = self.PS[1]
        self.mm(pr, pr[:, :], sel, sel[:, :], cumT, cumT[:, :, :].rearrange("p j h -> p (j h)"), start=True, stop=True)
        kb.op("dve", lambda: nc.vector.tensor_copy(out=refbc[:, :, :], in_=pr[:, :].rearrange("p (j h) -> p j h", h=16)),
              reads=[pr], writes=[refbc])
        self.barrier()
        tri = kb.sb("tri", [128, 128], BF16)
        tri32 = Buf(kb.sb("xo", [128, D], F32)[:, 0:128], "tri32")
        self.load(tri32, tri32[:, :], di["tri"], di["tri"][:, :])
        kb.op("dve", lambda: nc.vector.tensor_copy(out=tri[:, :], in_=tri32[:, :]), reads=[tri32], writes=[tri])
        onesk = kb.sb("onesk", [128, 128], BF16)
        kb.op("dve", lambda: nc.vector.memset(onesk[:, :], 1.0), writes=[onesk])
        KTh = [kb.sb(f"KTh{i}", [96, S], BF16) for i in range(2)]
        Qh = [kb.sb(f"Qh{i}", [96, S], BF16) for i in range(2)]
        Vp = [kb.sb(f"Vp{i}", [128, 32, 128], BF16) for i in range(2)]
        Bt = [kb.sb(f"Bt{i}", [128, 8, 32], F32) for i in range(2)]
        for i in range(2):
            kt_, q_ = KTh[i], Qh[i]
            kb.op("pool", lambda kt_=kt_: nc.gpsimd.memset(kt_[64:96, :], 0.0), writes=[kt_])
            kb.op("pool", lambda kt_=kt_: nc.gpsimd.memset(kt_[64:65, :], 1.0), writes=[kt_])
            kb.op("pool", lambda q_=q_: nc.gpsimd.memset(q_[64:96, :], 0.0), writes=[q_])
        self._attn_n = 0
        self._attn_par = 0
        for m in range(8):
            vp = Vp[m % 2]
            for jj in range(4):
                self.load(vp, vp[:, jj * 8:(jj + 1) * 8, :],
                          VC, VC[jj * 1024:(jj + 1) * 1024, m * 128:(m + 1) * 128].rearrange("(j p) d -> p j d", p=128))
            for hh in range(2):
                h = m * 2 + hh
                kt = KTh[h % 2]
                q = Qh[h % 2]
                self.load(kt, kt[0:64, :], KC, KC[m, hh * 64:(hh + 1) * 64, :])
                self.load(q, q[0:64, :], QC, QC[m, hh * 64:(hh + 1) * 64, :])
                self.load(q, q[64:65, :], self.CQ, self.CQ[h:h + 1, :])
                bt = Bt[h % 2]
                kb.op("dve", lambda bt=bt, h=h: nc.vector.tensor_tensor(
                    out=bt[:, :, :], in0=refbc[:, 3::4, h].unsqueeze(2).to_broadcast([128, 8, 32]),
                    in1=cumT[:, :, h].unsqueeze(1).to_broadcast([128, 8, 32]), op=ALU.subtract),
                    reads=[refbc, cumT], writes=[bt])

                def mask_fn(pt, cs, i, j):
                    if i == j:
                        kb.op("pool", lambda: nc.gpsimd.tensor_tensor(out=pt[:, cs:cs + 128], in0=pt[:, cs:cs + 128],
                                                                      in1=tri[:, :], op=ALU.mult), reads=[pt, tri], writes=[pt])
                for I in range(8):
                    outs = self.attn_block(I, kt, lambda j, kt=kt: [kt[:, j * 128:(j + 1) * 128]],
                                           q, lambda a, b, q=q: [q[:, a:b]],
                                           vp, lambda j, vp=vp, hh=hh: [vp[:, j, hh * 64:(hh + 1) * 64]], [64],
                                           lambda i, j, bt=bt: (bt, bt[:, i, j:j + 1]), mask_fn, onesk, clamp_diag=True)
                    self.load(OT, OT[h, :, I * 512:(I + 1) * 512], outs[0], outs[0][0:64, :])
        self.barrier()
        gv, sv, gate = self.load_modvecs(l, 0, "norm_mix")
        ots = [kb.sb(f"ots{i}", [64, 16, 128], BF16) for i in range(2)]

        def ot_loader(ti):
            ob = ots[ti % 2]
            self.load(ob, ob[:, :, :], OT, OT[:, :, ti * 128:(ti + 1) * 128].rearrange("h p t -> p h t"))
            return [(ob, ob[:, h, :]) for h in range(16)]
        self.outproj(l, XIN, XOUT, gate, di["odd_w_out"], lambda r0, K: di["odd_w_out"][o, r0:r0 + K, :],
                     [(64, h * 64) for h in range(16)], ot_loader)

    def phase_peer(self, l, XIN, XOUT):
        nc, kb, di = self.nc, self.kb, self.di
        gv, sv, gate = self.load_modvecs(l, 1, "norm_ffn")
        w16 = kb.sb("w16p", [128, 8, 2048], BF16)
        self.load_w16(w16, di["peer_w_q"], lambda kc: di["peer_w_q"][l, kc * 128:(kc + 1) * 128, :], 2048, stage="uvrow")
        self.barrier(reset=False)
        skT = kb.sb("skT", [128, 2, 128], F32)
        for p in range(2):
            self.load(skT, skT[:, p, :], di["peer_skT"], di["peer_skT"][l, p, :, :])
        iota16 = kb.sb("iota16", [128, 16], F32)
        self.load(iota16, iota16[:, :], di["iota16"], di["iota16"][:, :])
        hT = kb.sb("hT", [128, 8, 256], BF16)
        qT = kb.sb("qT", [128, 16, 256], F32)
        xts = [kb.sb(f"xt{i}", [128, D], F32) for i in range(2)]
        hts = [kb.sb(f"htok{i}", [128, D], F32) for i in range(2)]
        h16 = [kb.sb(f"h16_{i}", [128, D], BF16) for i in range(2)]
        ssb = kb.sb("ssb", [128, 16, 128], F32)
        tv = kb.sb("tv", [128, 16, 16], F32)
        tiu = kb.sb("tiu", [128, 16, 16], U32)
        tif = kb.sb("tif", [128, 16, 16], F32)
        cand = kb.sb("cand", [128, 8, 16, 16], F32)
        bv = kb.sb("bv", [128, 8, 16], F32)
        bpu = kb.sb("bpu", [128, 8, 16], U32)
        bi_u = kb.sb("bi_u", [128, 8, 16], U32)
        bj_u = kb.sb("bj_u", [128, 8, 16], U32)
        bi_f = kb.sb("bi_f", [128, 8, 16], F32)
        bj_f = kb.sb("bj_f", [128, 8, 16], F32)
        eq = kb.sb("eq", [128, 8, 16, 16], F32)
        tva = [Buf(tv[:, g, 0:8], f"tva{g}") for g in range(16)]
        tvb = [Buf(tv[:, g, 8:16], f"tvb{g}") for g in range(16)]
        tia = [Buf(tiu[:, g, 0:8], f"tia{g}") for g in range(16)]
        tib = [Buf(tiu[:, g, 8:16], f"tib{g}") for g in range(16)]
        s2g = [kb.sb(f"s2g{g}", [128, 128], F32) for g in range(16)]
        s2h = [kb.sb(f"s2h{g}", [128, 256], F32) for g in range(4)]
        bva = [Buf(bv[:, h, 0:8], f"bva{h}") for h in range(8)]
        bvb = [Buf(bv[:, h, 8:16], f"bvb{h}") for h in range(8)]
        bpa = [Buf(bpu[:, h, 0:8], f"bpa{h}") for h in range(8)]
        bpb = [Buf(bpu[:, h, 8:16], f"bpb{h}") for h in range(8)]
        n0 = kb.sb("n0", [128, 8, 16], F32)
        n1 = kb.sb("n1", [128, 8, 16], F32)
        ef = kb.sb("ef", [128, 128], F32)
        eidx = [kb.sb(f"eidx{i}", [128, 128], U32) for i in range(2)]
        gsum = kb.sb("gsum", [128, 8], F32)
        gat = kb.sb("gat", [128, 8, 16], F32)
        actv = kb.sb("actv", [128, 128], F32)
        t1 = kb.sb("t1", [128, 128], F32)
        xg = kb.sb("xg", [128, 128], F32)
        wgt = [kb.sb(f"wgt{i}", [128, 128], F32) for i in range(2)]
        NG = 12
        ug = [kb.sb(f"uvrow{i}", [128, 2 * D], BF16) for i in range(NG)]
        vs = [kb.sb(f"vs{i}", [128, D], BF16) for i in range(2)]
        junk2 = kb.sb("junk2", [128, D], BF16)
        xo = kb.sb("xo", [128, D], F32)
        self.convert_uv(l)
        UV = self.UV16L[l]
        gi = 0
        for blk in range(S // 256):
            for tt in range(2):
                ti = blk * 2 + tt
                xt = xts[tt]
                self.load(xt, xt[:, :], XIN, XIN[ti * 128:(ti + 1) * 128, :])
                self.norm_tile(xt, gv, sv, hts[tt], h16[tt % 2])
                self.transpose_tile(h16[tt % 2], hT, tt * 128)
            for g in range(16):
                pb = self.PS[g % 2]
                for kc in range(8):
                    self.mm(pb, pb[:, 0:256], w16, w16[:, kc, g * 128:(g + 1) * 128], hT, hT[:, kc, :],
                            start=(kc == 0), stop=(kc == 7))
                if g % 2 == 0:
                    kb.op("act", lambda pb=pb, g=g: nc.scalar.copy(out=qT[:, g, :], in_=pb[:, 0:256]), reads=[pb], writes=[qT])
                else:
                    kb.op("dve", lambda pb=pb, g=g: nc.vector.tensor_copy(out=qT[:, g, :], in_=pb[:, 0:256]), reads=[pb], writes=[qT])
            for tt in range(2):
                ti = blk * 2 + tt
                xt = xts[tt]
                htok = hts[tt]
                for g in range(16):
                    pb = self.PS[2 + g // 4]
                    self.mm(pb, pb[:, (g % 4) * 128:(g % 4 + 1) * 128], qT, qT[:, g, tt * 128:(tt + 1) * 128],
                            skT, skT[:, g % 2, :], start=True, stop=True)
                for q4 in range(4):
                    pb = self.PS[2 + q4]
                    kb.op("act", lambda pb=pb, q4=q4: nc.scalar.copy(
                        out=ssb[:, q4 * 4:(q4 + 1) * 4, :], in_=pb[:, :].rearrange("p (g n) -> p g n", g=4)),
                        reads=[pb], writes=[ssb])
                for g in range(16):
                    kb.op("dve", lambda g=g: nc.vector.max(out=tv[:, g, 0:8], in_=ssb[:, g, :]), reads=[ssb], writes=[tva[g]])
                for g in range(16):
                    kb.op("dve", lambda g=g: nc.vector.max_index(out=tiu[:, g, 0:8], in_max=tv[:, g, 0:8], in_values=ssb[:, g, :]),
                          reads=[ssb, tva[g]], writes=[tia[g]])
                for g in range(16):
                    kb.op("dve", lambda g=g: nc.vector.match_replace(out=s2g[g][:, :], in_to_replace=tv[:, g, 0:8],
                                                                     in_values=ssb[:, g, :], imm_value=-1e30),
                          reads=[ssb, tva[g]], writes=[s2g[g]])
                for g in range(16):
                    kb.op("dve", lambda g=g: nc.vector.max(out=tv[:, g, 8:16], in_=s2g[g][:, :]), reads=[s2g[g]], writes=[tvb[g]])
                for g in range(16):
                    kb.op("dve", lambda g=g: nc.vector.max_index(out=tiu[:, g, 8:16], in_max=tv[:, g, 8:16], in_values=s2g[g][:, :]),
                          reads=[s2g[g], tvb[g]], writes=[tib[g]])
                kb.op("dve", lambda: nc.vector.tensor_copy(out=tif[:, :, :], in_=tiu[:, :, :]), reads=tia + tib, writes=[tif])
                kb.op("dve", lambda: nc.vector.tensor_tensor(
                    out=cand[:, :, :, :], in0=tv[:, 0::2, :].unsqueeze(3).to_broadcast([128, 8, 16, 16]),
                    in1=tv[:, 1::2, :].unsqueeze(2).to_broadcast([128, 8, 16, 16]), op=ALU.add),
                    reads=tva + tvb, writes=[cand])
                for hb in range(2):
                    hs = range(hb * 4, hb * 4 + 4)
                    cfs = {h: cand[:, h, :, :].rearrange("p a b -> p (a b)") for h in hs}
                    for h in hs:
                        kb.op("dve", lambda h=h, cf=cfs[h]: nc.vector.max(out=bv[:, h, 0:8], in_=cf), reads=[cand], writes=[bva[h]])
                    for h in hs:
                        kb.op("dve", lambda h=h, cf=cfs[h]: nc.vector.max_index(out=bpu[:, h, 0:8], in_max=bv[:, h, 0:8], in_values=cf),
                              reads=[cand, bva[h]], writes=[bpa[h]])
                    for h in hs:
                        kb.op("dve", lambda h=h, cf=cfs[h]: nc.vector.match_replace(out=s2h[h % 4][:, :], in_to_replace=bv[:, h, 0:8],
                                                                         in_values=cf, imm_value=-1e30),
                              reads=[cand, bva[h]], writes=[s2h[h % 4]])
                    for h in hs:
                        kb.op("dve", lambda h=h: nc.vector.max(out=bv[:, h, 8:16], in_=s2h[h % 4][:, :]), reads=[s2h[h % 4]], writes=[bvb[h]])
                    for h in hs:
                        kb.op("dve", lambda h=h: nc.vector.max_index(out=bpu[:, h, 8:16], in_max=bv[:, h, 8:16], in_values=s2h[h % 4][:, :]),
                              reads=[s2h[h % 4], bvb[h]], writes=[bpb[h]])
                bvall = bva + bvb
                bpall = bpa + bpb
                kb.op("dve", lambda: nc.vector.tensor_scalar(out=bi_u[:, :, :], in0=bpu[:, :, :], scalar1=4, scalar2=None,
                                                             op0=ALU.logical_shift_right), reads=bpall, writes=[bi_u])
                kb.op("dve", lambda: nc.vector.tensor_scalar(out=bj_u[:, :, :], in0=bpu[:, :, :], scalar1=15, scalar2=None,
                                                             op0=ALU.bitwise_and), reads=bpall, writes=[bj_u])
                kb.op("dve", lambda: nc.vector.tensor_copy(out=bi_f[:, :, :], in_=bi_u[:, :, :]), reads=[bi_u], writes=[bi_f])
                kb.op("dve", lambda: nc.vector.tensor_copy(out=bj_f[:, :, :], in_=bj_u[:, :, :]), reads=[bj_u], writes=[bj_f])
                io_b = iota16[:, :].unsqueeze(1).unsqueeze(1).to_broadcast([128, 8, 16, 16])
                for (bf, par, nn) in ((bi_f, 0, n0), (bj_f, 1, n1)):
                    kb.op("dve", lambda bf=bf: nc.vector.tensor_tensor(
                        out=eq[:, :, :, :], in0=bf[:, :, :].unsqueeze(3).to_broadcast([128, 8, 16, 16]), in1=io_b,
                        op=ALU.is_equal), reads=[bf, iota16], writes=[eq])
                    kb.op("dve", lambda par=par: nc.vector.tensor_tensor(
                        out=eq[:, :, :, :], in0=eq[:, :, :, :],
                        in1=tif[:, par::2, :].unsqueeze(2).to_broadcast([128, 8, 16, 16]), op=ALU.mult),
                        reads=[eq, tif], writes=[eq])
                    kb.op("dve", lambda nn=nn: nc.vector.tensor_reduce(out=nn[:, :, :], in_=eq[:, :, :, :], axis=AX.X, op=ALU.add),
                          reads=[eq], writes=[nn])
                kb.op("dve", lambda: nc.vector.scalar_tensor_tensor(
                    out=ef[:, :], in0=n0[:, :, :].rearrange("p a b -> p (a b)"), scalar=128.0,
                    in1=n1[:, :, :].rearrange("p a b -> p (a b)"), op0=ALU.mult, op1=ALU.add),
                    reads=[n0, n1], writes=[ef])
                if l > 0:
                    kb.op("dve", lambda: nc.vector.tensor_scalar(out=ef[:, :], in0=ef[:, :], scalar1=float(l * 16384), scalar2=None,
                                                                 op0=ALU.add), reads=[ef], writes=[ef])
                ei = eidx[ti % 2]
                kb.op("dve", lambda ei=ei: nc.vector.tensor_copy(out=ei[:, :], in_=ef[:, :]), reads=[ef], writes=[ei])
                kb.op("dve", lambda: nc.vector.tensor_tensor(
                    out=gat[:, :, :], in0=bv[:, :, :], in1=bv[:, :, 0:1].to_broadcast([128, 8, 16]), op=ALU.subtract),
                    reads=bvall, writes=[gat])
                kb.op("act", lambda: nc.scalar.activation(out=gat[:, :, :], in_=gat[:, :, :], func=AF.Exp),
                      reads=[gat], writes=[gat])
                kb.op("dve", lambda: nc.vector.tensor_reduce(out=gsum[:, :], in_=gat[:, :, :], axis=AX.X, op=ALU.add),
                      reads=[gat], writes=[gsum])
                kb.op("dve", lambda: nc.vector.reciprocal(out=gsum[:, :], in_=gsum[:, :]), reads=[gsum], writes=[gsum])
                kb.op("dve", lambda: nc.vector.tensor_tensor(
                    out=gat[:, :, :], in0=gat[:, :, :], in1=gsum[:, :].unsqueeze(2).to_broadcast([128, 8, 16]), op=ALU.mult),
                    reads=[gat, gsum], writes=[gat])
                C1 = 0.044715
                C2 = 2.0 * math.sqrt(2.0 / math.pi)
                wg = wgt[ti % 2]
                h16t = h16[tt % 2]
                gatf = gat[:, :, :].rearrange("p a b -> p (a b)")
                po0, po1 = self.PS[0], self.PS[1]
                def tail(g4, rows):
                    c_ = slice(g4 * 4, g4 * 4 + 4)
                    kb.op("dve", lambda: nc.vector.tensor_tensor(out=wg[:, c_], in0=t1[:, c_], in1=xg[:, c_], op=ALU.mult),
                          reads=[t1, xg], writes=[wg])
                    for k in range(4):
                        sl = g4 * 4 + k
                        u = rows[k]
                        vsb = vs[sl % 2]
                        kb.op("act", lambda u=u, vsb=vsb, sl=sl: nc.scalar.activation(
                            out=vsb[:, :], in_=u[:, D:2 * D], func=AF.Copy, scale=wg[:, sl:sl + 1]),
                            reads=[u, wg], writes=[vsb])
                        self.mm(po0, po0[:, :], self.identb, self.identb[:, :], vsb, vsb[:, 0:512], start=(sl == 0), stop=(sl == 127))
                        self.mm(po1, po1[:, :], self.identb, self.identb[:, :], vsb, vsb[:, 512:1024], start=(sl == 0), stop=(sl == 127))

                pending = None
                for g4 in range(32):
                    rows = []
                    for k in range(4):
                        sl = g4 * 4 + k
                        u = ug[gi % NG]
                        gi += 1
                        rows.append(u)
                        kb.dma_op("pool", lambda u=u, ei=ei, sl=sl: nc.gpsimd.indirect_dma_start(
                            out=u[:, :], out_offset=None, in_=UV[:, :],
                            in_offset=bass.IndirectOffsetOnAxis(ap=ei[:, sl:sl + 1], axis=0)),
                            reads=[UV, ei], writes=[u])
                        kb.op("dve", lambda u=u, sl=sl, h16t=h16t: nc.vector.scalar_tensor_tensor(
                            out=junk2[:, :], in0=u[:, 0:D], scalar=1.0, in1=h16t[:, :], op0=ALU.mult, op1=ALU.mult,
                            accum_out=actv[:, sl:sl + 1]), reads=[u, h16t], writes=[junk2, actv])
                    c_ = slice(g4 * 4, g4 * 4 + 4)
                    kb.op("dve", lambda c_=c_: nc.vector.tensor_tensor(out=t1[:, c_], in0=actv[:, c_], in1=actv[:, c_], op=ALU.mult),
                          reads=[actv], writes=[t1])
                    kb.op("dve", lambda c_=c_: nc.vector.tensor_scalar(out=t1[:, c_], in0=t1[:, c_], scalar1=C1, scalar2=1.0,
                                                                       op0=ALU.mult, op1=ALU.add), reads=[t1], writes=[t1])
                    kb.op("dve", lambda c_=c_: nc.vector.tensor_tensor(out=t1[:, c_], in0=t1[:, c_], in1=actv[:, c_], op=ALU.mult),
                          reads=[t1, actv], writes=[t1])
                    kb.op("dve", lambda c_=c_: nc.vector.tensor_tensor(out=xg[:, c_], in0=actv[:, c_], in1=gatf[:, c_], op=ALU.mult),
                          reads=[actv, gat], writes=[xg])
                    kb.op("act", lambda c_=c_: nc.scalar.activation(out=t1[:, c_], in_=t1[:, c_], func=AF.Sigmoid, scale=C2),
                          reads=[t1], writes=[t1])
                    if pending is not None:
                        tail(*pending)
                    pending = (g4, rows)
                tail(*pending)
                kb.op("dve", lambda: nc.vector.tensor_tensor(out=xo[:, 0:512], in0=po0[:, :], in1=gate[:, 0:512], op=ALU.mult),
                      reads=[po0, gate], writes=[xo])
                kb.op("dve", lambda: nc.vector.tensor_tensor(out=xo[:, 512:1024], in0=po1[:, :], in1=gate[:, 512:1024], op=ALU.mult),
                      reads=[po1, gate], writes=[xo])
                kb.op("dve", lambda xt=xt: nc.vector.tensor_tensor(out=xo[:, :], in0=xo[:, :], in1=xt[:, :], op=ALU.add),
                      reads=[xo, xt], writes=[xo])
                self.load(XOUT, XOUT[ti * 128:(ti + 1) * 128, :], xo, xo[:, :])

    def phase_final(self, XIN, XOUT, do_norm):
        nc, kb, di = self.nc, self.kb, self.di
        nv = kb.sb("gvec", [128, D], F32)
        zv = kb.sb("shvec", [128, D], F32)
        if do_norm:
            self.load(nv, nv[:, :], di["norm_final"], di["norm_final"][:, :].partition_broadcast(128))
            kb.op("dve", lambda: nc.vector.memset(zv[:, :], 0.0), writes=[zv])
        xts = [kb.sb(f"xt{i}", [128, D], F32) for i in range(2)]
        hts = [kb.sb(f"htok{i}", [128, D], F32) for i in range(2)]
        for ti in range(NT):
            xt = xts[ti % 2]
            self.load(xt, xt[:, :], XIN, XIN[ti * 128:(ti + 1) * 128, :])
            if do_norm:
                self.norm_tile(xt, nv, zv, hts[ti % 2], None)
                self.load(XOUT, XOUT[ti * 128:(ti + 1) * 128, :], hts[ti % 2], hts[ti % 2][:, :])
            else:
                self.load(XOUT, XOUT[ti * 128:(ti + 1) * 128, :], xt, xt[:, :])

    def build(self):
        self.phase_mods()
        self.barrier()
        cur = self.di["x"]
        pp = [self.XA, self.XB]
        n = 0
        for l in self.layers:
            if not self.dbg_peer_only:
                nxt = pp[n % 2]; n += 1
                self.phase_mixer(l, cur, nxt)
                self.barrier()
                cur = nxt
            nxt = pp[n % 2]; n += 1
            self.phase_peer(l, cur, nxt)
            self.barrier()
            cur = nxt
        self.phase_final(cur, self.out, self.final_norm)
        self.kb.finish([self.out])
        self.kb.emit()
        return self.nc


def _bucket_idx(dist):
    max_exact = 16
    d = np.maximum(dist, 0)
    df = np.maximum(d, 1).astype(np.float32)
    large = max_exact + (np.log(df / max_exact) / np.float32(math.log(128 / max_exact)) * (32 - max_exact)).astype(np.int32)
    large = np.minimum(large, 31)
    return np.where(d < max_exact, d, large)


def _bias_tiles(rb):
    s_ = np.arange(128)[:, None]
    t_ = np.arange(128)[None, :]
    out = np.zeros((128, 24, 128), dtype=np.float32)
    for dl in range(2):
        idx = _bucket_idx(128 * dl + t_ - s_)
        for h in range(12):
            out[:, h * 2 + dl, :] = rb[idx, h]
    return out


class _IM(dict):
    def __setitem__(self, k, v):
        super().__setitem__(k if k.startswith("i_") else "i_" + k, v)


def host_shared(inputs):
    f = np.float32
    u = np.asarray(inputs["peer_u"], dtype=f).reshape(DEPTH * 16384, D)
    v = np.asarray(inputs["peer_v"], dtype=f).reshape(DEPTH * 16384, D)
    return {"peer_uv": np.ascontiguousarray(np.concatenate([u, v], axis=1))}


def host_inputs(inputs, b, shared=None):
    f = np.float32
    if shared is None:
        shared = host_shared(inputs)
    m = _IM()
    m["x"] = np.ascontiguousarray(inputs["x"][b], dtype=f)
    m["ccol"] = np.ascontiguousarray(inputs["c"][b].reshape(8, 128).T, dtype=f)
    m["ident"] = np.eye(128, dtype=f)
    m["ada_w"] = np.asarray(inputs["ada_w"], dtype=f)
    m["ada_b"] = np.asarray(inputs["ada_b"], dtype=f).reshape(DEPTH, 1, 6 * D)
    m["norm_mix"] = np.asarray(inputs["norm_mix"], dtype=f).reshape(DEPTH, 1, D)
    m["norm_ffn"] = np.asarray(inputs["norm_ffn"], dtype=f).reshape(DEPTH, 1, D)
    m["norm_final"] = np.asarray(inputs["norm_final"], dtype=f).reshape(1, D)
    m["peer_w_q"] = np.asarray(inputs["peer_w_q"], dtype=f)
    m["peer_skT"] = np.ascontiguousarray(np.transpose(np.asarray(inputs["peer_sub_keys"], dtype=f), (0, 1, 3, 2)))
    m["peer_uv"] = shared["peer_uv"]
    m["odd_w_in"] = np.asarray(inputs["odd_w_in"], dtype=f)
    m["odd_b_forget"] = np.asarray(inputs["odd_b_forget"], dtype=f).reshape(2, 16, 1)
    m["odd_w_out"] = np.asarray(inputs["odd_w_out"], dtype=f)
    m["even_w_in"] = np.asarray(inputs["even_w_in"], dtype=f)
    m["even_w_out"] = np.asarray(inputs["even_w_out"], dtype=f)
    m["a_kv_norm"] = np.asarray(inputs["a_kv_norm"], dtype=f).reshape(2, 1, 256)
    m["ukT"] = np.ascontiguousarray(np.transpose(np.asarray(inputs["a_w_uk"], dtype=f), (0, 2, 3, 1)))
    m["a_w_uv"] = np.asarray(inputs["a_w_uv"], dtype=f).reshape(2, 256, 512)
    m["b_lambda"] = np.asarray(inputs["b_lambda"], dtype=f)
    m["b_subln"] = np.asarray(inputs["b_subln"], dtype=f).reshape(2, 128, 1)
    rb = np.asarray(inputs["rel_bias"], dtype=f)
    m["b31rep"] = np.ascontiguousarray(np.broadcast_to(rb[31:32, :], (128, 12)))
    m["biasT"] = _bias_tiles(rb)
    m["negtri"] = np.ascontiguousarray((np.tril(np.ones((128, 128), dtype=f), -1).T * -1e30).astype(f))
    sel = np.zeros((128, 128), dtype=f); sel[127, :] = 1.0
    m["sel127"] = sel
    m["tri"] = np.triu(np.ones((128, 128), dtype=f))
    m["iota16"] = np.ascontiguousarray(np.broadcast_to(np.arange(16, dtype=f)[None, :], (128, 16)))
    return m


def kernel(**inputs):
    prog = Prog(layers=list(range(DEPTH)), final_norm=True)
    nc = prog.build()
    ncores = 4
    shared = host_shared(inputs)
    in_maps = [host_inputs(inputs, b, shared) for b in range(ncores)]
    res = run_bass_kernel_spmd(nc, in_maps, core_ids=list(range(ncores)))
    return np.stack([np.asarray(r["out"], dtype=np.float32) for r in res.results], axis=0)
```
